# Optimizing a Trainium2 kernel written in Bass

```python
import math
import jax
import jax.numpy as jnp
from jax import lax
import numpy as np

D_MODEL = 1024
BATCH = 1
SEQ = 16384
DEPTH = 1

CHUNK = 64
EPS = 1e-6
D_CONV = D_MODEL
CONV_K = 31
SSM_EXPAND = 2
D_INNER = SSM_EXPAND * D_MODEL
SSM_HEADDIM = 64
SSM_HEADS = D_INNER // SSM_HEADDIM
SSM_GROUPS = 4
SSM_HPG = SSM_HEADS // SSM_GROUPS
SSM_STATE = 128
SSM_CONV_K = 4
N_BRANCHES = 2
IN_GLU = 2 * D_CONV
IN_Z = D_INNER
IN_XBC = D_INNER + 2 * SSM_GROUPS * SSM_STATE
IN_DT = SSM_HEADS
IN_GATE = N_BRANCHES * D_MODEL
D_IN_PROJ = IN_GLU + IN_Z + IN_XBC + IN_DT + IN_GATE
MOE_GROUPS = 8
MOE_EXPERTS_PER_GROUP = 8
N_EXPERTS = MOE_GROUPS * MOE_EXPERTS_PER_GROUP
MOE_TOP_K = 2
D_FF_EXPERT = D_MODEL // 2
MOE_BLOCK = 128

kernel_name = 'hybrid_conformer_ssd_hmoe_block'


def _rmsnorm(x, g):
    xf = x.astype(jnp.float32)
    y = xf * lax.rsqrt(jnp.mean(xf * xf, axis=-1, keepdims=True) + EPS)
    return (y * g.astype(jnp.float32)).astype(x.dtype)


def _layernorm(x, g, b):
    xf = x.astype(jnp.float32)
    mu = jnp.mean(xf, axis=-1, keepdims=True)
    var = jnp.mean(jnp.square(xf - mu), axis=-1, keepdims=True)
    y = (xf - mu) * lax.rsqrt(var + EPS)
    return (y * g.astype(jnp.float32) + b.astype(jnp.float32)).astype(x.dtype)


def _causal_dwconv(x, w, b):
    k = w.shape[0]
    xp = jnp.pad(x, ((0, 0), (k - 1, 0), (0, 0)))
    y = lax.conv_general_dilated(xp, w[:, None, :].astype(x.dtype), (1,), 'VALID',
                                 dimension_numbers=('NWC', 'WIO', 'NWC'),
                                 feature_group_count=x.shape[-1])
    return y + b.astype(x.dtype)


def _ssd(xh, dt, a, bm, cm):
    bsz, seq = xh.shape[0], xh.shape[1]
    nc = seq // CHUNK
    x = (xh * dt[..., None]).reshape(bsz, nc, CHUNK, SSM_GROUPS, SSM_HPG, SSM_HEADDIM)
    ad = (dt * a).reshape(bsz, nc, CHUNK, SSM_GROUPS, SSM_HPG)
    bc = bm.reshape(bsz, nc, CHUNK, SSM_GROUPS, SSM_STATE)
    cc = cm.reshape(bsz, nc, CHUNK, SSM_GROUPS, SSM_STATE)
    a_cs = jnp.cumsum(ad, axis=2)
    causal = jnp.tril(jnp.ones((CHUNK, CHUNK), dtype=bool))[:, :, None, None]
    seg = a_cs[:, :, :, None] - a_cs[:, :, None, :]
    lmat = jnp.exp(jnp.where(causal, seg, -jnp.inf))
    cb = jnp.einsum('bclgn,bcsgn->bclsg', cc, bc)
    y_diag = jnp.einsum('bclsgr,bcsgrp->bclgrp', cb[..., None] * lmat, x)
    decay = jnp.exp(a_cs[:, :, -1:] - a_cs)
    states = jnp.einsum('bclgn,bclgrp->bcgrpn', bc, x * decay[..., None])
    chunk_decay = jnp.exp(a_cs[:, :, -1])

    def step(carry, inp):
        st, dec = inp
        return carry * dec[..., None, None] + st, carry

    init = jnp.zeros((bsz, SSM_GROUPS, SSM_HPG, SSM_HEADDIM, SSM_STATE), jnp.float32)
    _, prev = lax.scan(step, init, (jnp.moveaxis(states, 1, 0), jnp.moveaxis(chunk_decay, 1, 0)))
    prev = jnp.moveaxis(prev, 0, 1)
    y_off = jnp.einsum('bclgn,bcgrpn->bclgrp', cc, prev) * jnp.exp(a_cs)[..., None]
    return (y_diag + y_off).reshape(bsz, seq, SSM_HEADS, SSM_HEADDIM)


def _mixer(n, w_in, cv_dw_w, cv_dw_b, cv_ln_g, cv_ln_b, w_cv_out, ssm_conv_w, ssm_conv_b,
           dt_bias, a_log, d_skip, ssm_norm_g, w_ssm_out, w_mix_out):
    bsz, seq, _ = n.shape
    f32 = jnp.float32
    cuts = [IN_GLU, IN_GLU + IN_Z, IN_GLU + IN_Z + IN_XBC, IN_GLU + IN_Z + IN_XBC + IN_DT]
    glu_in, z, xbc, dt_raw, gate_in = jnp.split(n @ w_in, cuts, axis=-1)
    u_a, u_b = jnp.split(glu_in, 2, axis=-1)
    u = u_a * jax.nn.sigmoid(u_b)
    u = jax.nn.silu(_layernorm(_causal_dwconv(u, cv_dw_w, cv_dw_b), cv_ln_g, cv_ln_b))
    y_cv = u @ w_cv_out
    xbc = jax.nn.silu(_causal_dwconv(xbc, ssm_conv_w, ssm_conv_b))
    xs, bm, cm = jnp.split(xbc, [D_INNER, D_INNER + SSM_GROUPS * SSM_STATE], axis=-1)
    dt = jax.nn.softplus(dt_raw.astype(f32) + dt_bias.astype(f32))
    a = -jnp.exp(a_log.astype(f32))
    xh = xs.reshape(bsz, seq, SSM_HEADS, SSM_HEADDIM).astype(f32)
    y = _ssd(xh, dt, a,
             bm.reshape(bsz, seq, SSM_GROUPS, SSM_STATE).astype(f32),
             cm.reshape(bsz, seq, SSM_GROUPS, SSM_STATE).astype(f32))
    y = y + d_skip.astype(f32)[:, None] * xh
    y = y.reshape(bsz, seq, D_INNER).astype(n.dtype) * jax.nn.silu(z)
    gs = D_INNER // SSM_GROUPS
    y = _rmsnorm(y.reshape(bsz, seq, SSM_GROUPS, gs), ssm_norm_g.reshape(SSM_GROUPS, gs))
    y_ssm = y.reshape(bsz, seq, D_INNER) @ w_ssm_out
    g_cv, g_ssm = jnp.split(jax.nn.sigmoid(gate_in), 2, axis=-1)
    return (g_cv * y_cv + g_ssm * y_ssm) @ w_mix_out


def _hier_moe(xt, w_grp, b_grp, w_er, b_er, w1, w3, w2):
    t, d = xt.shape
    f32 = jnp.float32
    gp = jax.nn.softmax((xt @ w_grp + b_grp).astype(f32), axis=-1)
    g_p, g_idx = lax.top_k(gp, 1)
    el = (xt @ w_er + b_er).astype(f32).reshape(t, MOE_GROUPS, MOE_EXPERTS_PER_GROUP)
    idx = jnp.broadcast_to(g_idx[:, :, None], (t, 1, MOE_EXPERTS_PER_GROUP))
    el_sel = jnp.take_along_axis(el, idx, axis=1)[:, 0]
    ep = jax.nn.softmax(el_sel, axis=-1)
    e_p, e_loc = lax.top_k(ep, MOE_TOP_K)
    e_p = e_p / jnp.sum(e_p, axis=-1, keepdims=True)
    weights = g_p * e_p
    e_glob = g_idx * MOE_EXPERTS_PER_GROUP + e_loc
    n_asg = t * MOE_TOP_K
    e_flat = e_glob.reshape(-1)
    tok_flat = jnp.repeat(jnp.arange(t, dtype=jnp.int32), MOE_TOP_K)
    w_flat = weights.reshape(-1)
    order = jnp.argsort(e_flat)
    e_sorted = e_flat[order]
    counts = jnp.bincount(e_flat, length=N_EXPERTS)
    starts = jnp.cumsum(counts) - counts
    pcounts = (counts + MOE_BLOCK - 1) // MOE_BLOCK * MOE_BLOCK
    pends = jnp.cumsum(pcounts)
    pstarts = pends - pcounts
    dest = pstarts[e_sorted] + (jnp.arange(n_asg) - starts[e_sorted])
    n_blocks = -(-n_asg // MOE_BLOCK) + N_EXPERTS
    n_rows = n_blocks * MOE_BLOCK
    buf_tok = jnp.zeros((n_rows,), jnp.int32).at[dest].set(tok_flat[order])
    buf_w = jnp.zeros((n_rows,), xt.dtype).at[dest].set(w_flat[order].astype(xt.dtype))
    blk_e = jnp.minimum(jnp.searchsorted(pends, jnp.arange(n_blocks) * MOE_BLOCK, side='right'),
                        N_EXPERTS - 1)
    xb = xt[buf_tok].reshape(n_blocks, MOE_BLOCK, d)

    def expert_block(args):
        xblk, e = args
        hdn = jax.nn.silu(xblk @ w1[e]) * (xblk @ w3[e])
        return hdn @ w2[e]

    yb = lax.map(expert_block, (xb, blk_e)).reshape(n_rows, d)
    return jnp.zeros_like(xt).at[buf_tok].add(yb * buf_w[:, None])


def setup_inputs(seed: int = 0) -> dict:
    key = jax.random.key(seed)
    ks = jax.random.split(key, 30)
    f = jnp.float32
    L = DEPTH

    def nrm(k, shape, scale):
        return jax.random.normal(k, shape, f) * scale

    dt0 = jnp.exp(jax.random.uniform(ks[14], (L, SSM_HEADS), f, math.log(1e-3), math.log(1e-1)))
    return {
        'x': nrm(ks[0], (BATCH, SEQ, D_MODEL), 1.0),
        'c': nrm(ks[1], (BATCH, D_MODEL), 1.0),
        'w_ada': nrm(ks[2], (L, D_MODEL, 6 * D_MODEL), 0.5 * D_MODEL ** -0.5),
        'b_ada': nrm(ks[3], (L, 6 * D_MODEL), 0.02),
        'norm1_g': 1.0 + nrm(ks[4], (L, D_MODEL), 0.02),
        'w_in': nrm(ks[5], (L, D_MODEL, D_IN_PROJ), D_MODEL ** -0.5),
        'cv_dw_w': nrm(ks[6], (L, CONV_K, D_CONV), CONV_K ** -0.5),
        'cv_dw_b': nrm(ks[7], (L, D_CONV), 0.02),
        'cv_ln_g': 1.0 + nrm(ks[8], (L, D_CONV), 0.02),
        'cv_ln_b': nrm(ks[9], (L, D_CONV), 0.02),
        'w_cv_out': nrm(ks[10], (L, D_CONV, D_MODEL), D_CONV ** -0.5),
        'ssm_conv_w': nrm(ks[11], (L, SSM_CONV_K, IN_XBC), SSM_CONV_K ** -0.5),
        'ssm_conv_b': nrm(ks[12], (L, IN_XBC), 0.02),
        'dt_bias': dt0 + jnp.log(-jnp.expm1(-dt0)),
        'a_log': jnp.log(jax.random.uniform(ks[15], (L, SSM_HEADS), f, 1.0, 16.0)),
        'd_skip': 1.0 + nrm(ks[16], (L, SSM_HEADS), 0.1),
        'ssm_norm_g': 1.0 + nrm(ks[17], (L, D_INNER), 0.02),
        'w_ssm_out': nrm(ks[18], (L, D_INNER, D_MODEL), D_INNER ** -0.5),
        'w_mix_out': nrm(ks[19], (L, D_MODEL, D_MODEL), D_MODEL ** -0.5),
        'norm2_g': 1.0 + nrm(ks[20], (L, D_MODEL), 0.02),
        'w_grp': nrm(ks[21], (L, D_MODEL, MOE_GROUPS), D_MODEL ** -0.5),
        'b_grp': nrm(ks[22], (L, MOE_GROUPS), 0.01),
        'w_er': nrm(ks[23], (L, D_MODEL, N_EXPERTS), D_MODEL ** -0.5),
        'b_er': nrm(ks[24], (L, N_EXPERTS), 0.01),
        'w1': nrm(ks[25], (L, N_EXPERTS, D_MODEL, D_FF_EXPERT), D_MODEL ** -0.5),
        'w3': nrm(ks[26], (L, N_EXPERTS, D_MODEL, D_FF_EXPERT), D_MODEL ** -0.5),
        'w2': nrm(ks[27], (L, N_EXPERTS, D_FF_EXPERT, D_MODEL), D_FF_EXPERT ** -0.5),
        'final_g': 1.0 + nrm(ks[28], (D_MODEL,), 0.02),
    }


def reference(x, c, w_ada, b_ada, norm1_g, w_in, cv_dw_w, cv_dw_b, cv_ln_g, cv_ln_b, w_cv_out,
              ssm_conv_w, ssm_conv_b, dt_bias, a_log, d_skip, ssm_norm_g, w_ssm_out, w_mix_out,
              norm2_g, w_grp, b_grp, w_er, b_er, w1, w3, w2, final_g):
    bsz, seq, d = x.shape
    h = x
    for i in range(DEPTH):
        mod = jax.nn.silu(c) @ w_ada[i] + b_ada[i]
        sh1, sc1, g1, sh2, sc2, g2 = jnp.split(mod[:, None, :], 6, axis=-1)
        n1 = _rmsnorm(h, norm1_g[i]) * (1.0 + sc1) + sh1
        h = h + g1 * _mixer(n1, w_in[i], cv_dw_w[i], cv_dw_b[i], cv_ln_g[i], cv_ln_b[i], w_cv_out[i],
                            ssm_conv_w[i], ssm_conv_b[i], dt_bias[i], a_log[i], d_skip[i],
                            ssm_norm_g[i], w_ssm_out[i], w_mix_out[i])
        n2 = _rmsnorm(h, norm2_g[i]) * (1.0 + sc2) + sh2
        moe = _hier_moe(n2.reshape(bsz * seq, d), w_grp[i], b_grp[i], w_er[i], b_er[i],
                        w1[i], w3[i], w2[i])
        h = h + g2 * moe.reshape(bsz, seq, d)
    return _rmsnorm(h, final_g)
```

```python
import functools
import contextlib
import numpy as np
import concourse.bass as bass
import concourse.mybir as mybir
from concourse.bass_utils import run_bass_kernel_spmd

F32 = mybir.dt.float32
BF16 = mybir.dt.bfloat16
I32 = mybir.dt.int32
U32 = mybir.dt.uint32
AF = mybir.ActivationFunctionType
ALU = mybir.AluOpType
AX = mybir.AxisListType

NCORES = 8
D = 1024
SEQ = 16384
T = SEQ // NCORES
NT = T // 128
HALO = 32
TH = T + HALO
DIN = 9248
C_GLU_A, C_GLU_B, C_Z, C_XBC, C_DT, C_GCV, C_GSSM = 0, 1024, 2048, 4096, 7168, 7200, 8224
EPS = 1e-6
ENGS = ("pe", "act", "dve", "pool", "sp")


class Buf:
    __slots__ = ("name", "w", "r")

    def __init__(self, name):
        self.name = name
        self.w = None
        self.r = []


class Sched:
    NDMA = 6

    def __init__(self, nc):
        self.nc = nc
        self.q = {e: [] for e in ENGS}
        self.sem = {e: nc.alloc_semaphore("prog_" + e) for e in ENGS}
        self.cnt = {e: 0 for e in ENGS}
        self.waited = {e: {} for e in ENGS}
        self.dq = ("sp", "pool", "act")
        self.dsem = {e: [nc.alloc_semaphore("dma_%s_%d" % (e, j)) for j in range(self.NDMA)]
                     for e in self.dq}
        self.dcnt = {e: [0] * self.NDMA for e in self.dq}
        self.didx = {e: 0 for e in self.dq}
        self.semkey = {}

    def _key(self, sem):
        k = id(sem)
        self.semkey[k] = sem
        return k

    def _waits(self, eng, reads, writes, skip_self):
        need = {}

        def add(ev):
            if ev is None:
                return
            sem, val = ev
            if skip_self and sem is self.sem[eng]:
                return
            k = self._key(sem)
            if need.get(k, 0) < val:
                need[k] = val
        for b in reads:
            add(b.w)
        for b in writes:
            add(b.w)
            for ev in b.r:
                add(ev)
        out = []
        wd = self.waited[eng]
        for k, val in need.items():
            if wd.get(k, 0) >= val:
                continue
            wd[k] = val
            out.append((self.semkey[k], val))
        return out

    def _emit_waits(self, eng, evs):
        for sem, val in evs:
            self.q[eng].append(functools.partial(lambda E, s, v: E.wait_ge(s, v), s=sem, v=val))

    def _record(self, ev, reads, writes):
        for b in reads:
            b.r.append(ev)
            if len(b.r) > 64:
                b.r = self._compact(b.r)
        for b in writes:
            b.w = ev
            b.r = []

    def _compact(self, evs):
        best = {}
        for sem, val in evs:
            k = self._key(sem)
            if best.get(k, 0) < val:
                best[k] = val
        return [(self.semkey[k], v) for k, v in best.items()]

    def op(self, eng, fn, reads=(), writes=()):
        evs = self._waits(eng, reads, writes, skip_self=(eng == "pe"))
        self._emit_waits(eng, evs)
        self.cnt[eng] += 1
        sem = self.sem[eng]
        self.q[eng].append(functools.partial(lambda E, f, s: f(E).then_inc(s, 1), f=fn, s=sem))
        ev = (sem, self.cnt[eng])
        self._record(ev, reads, writes)
        return ev

    def dma(self, eng, fn, reads=(), writes=()):
        j = self.didx[eng]
        self.didx[eng] = (j + 1) % self.NDMA
        sem = self.dsem[eng][j]
        prev = self.dcnt[eng][j]
        evs = self._waits(eng, reads, writes, skip_self=False)
        k = self._key(sem)
        if prev > 0 and self.waited[eng].get(k, 0) < 16 * prev:
            self.waited[eng][k] = 16 * prev
            evs.append((sem, 16 * prev))
        self._emit_waits(eng, evs)
        self.dcnt[eng][j] = prev + 1
        self.q[eng].append(functools.partial(lambda E, f, s: f(E).then_inc(s, 16), f=fn, s=sem))
        ev = (sem, 16 * (prev + 1))
        self._record(ev, reads, writes)
        return ev

    def barrier(self):
        evs = [(self.sem[e], self.cnt[e]) for e in ENGS if self.cnt[e] > 0]
        for e in self.dq:
            for j in range(self.NDMA):
                if self.dcnt[e][j] > 0:
                    evs.append((self.dsem[e][j], 16 * self.dcnt[e][j]))
        for eng in ENGS:
            wd = self.waited[eng]
            todo = []
            for sem, val in evs:
                if sem is self.sem[eng] and eng == "pe":
                    continue
                k = self._key(sem)
                if wd.get(k, 0) < val:
                    wd[k] = val
                    todo.append((sem, val))
            self._emit_waits(eng, todo)

    def finish(self, out_bufs):
        evs = self._waits("sp", out_bufs, (), skip_self=False)
        self._emit_waits("sp", evs)
        nc = self.nc
        q = self.q
        with nc.Block() as block:
            @block.tensor
            def _(E):
                for f in q["pe"]:
                    f(E)

            @block.scalar
            def _(E):
                for f in q["act"]:
                    f(E)

            @block.vector
            def _(E):
                for f in q["dve"]:
                    f(E)

            @block.gpsimd
            def _(E):
                for f in q["pool"]:
                    f(E)

            @block.sync
            def _(E):
                for f in q["sp"]:
                    f(E)


class Scope:
    _n = [0]

    def __init__(self, nc, S):
        self.nc = nc
        self.S = S
        self.es = contextlib.ExitStack()

    def __enter__(self):
        self.es.__enter__()
        return self

    def __exit__(self, *a):
        self.S.barrier()
        return self.es.__exit__(*a)

    def sb(self, name, shape, dt):
        Scope._n[0] += 1
        t = self.es.enter_context(self.nc.sbuf_tensor("%s_%d" % (name, Scope._n[0]), list(shape), dt))
        return t, Buf(name)


NEGV = -30000.0
BIG = 1.0e4
C_B = C_XBC + 2048
C_C = C_XBC + 2560


MOEQ = ("pool", "pool", "pool")


def build_program(stage="full", npre=NCORES - 1, nexp=64):
    nc = bass.Bass("TRN2", target_bir_lowering=False)
    S = Sched(nc)

    def din(name, shape, dt=F32):
        return nc.dram_tensor(name, list(shape), dt, kind="ExternalInput").ap()

    def dscr(name, shape, dt):
        return nc.dram_tensor(name, list(shape), dt, kind="Internal").ap()

    def wview(w, c0, n):
        return w[:, c0:c0 + n].rearrange("(k p) n -> p k n", p=128)

    def APX(ap, dims):
        return bass.AP(ap.tensor, ap.offset, [list(ap.ap[0])] + [list(d) for d in dims])

    def bc_in(ap, n):
        return APX(ap, [list(ap.ap[1]), [0, n]])

    def bc_mid(ap, r):
        return APX(ap, [[0, r], list(ap.ap[1])])

    def v3(ap, a, b):
        s = ap.ap[1][0]
        return APX(ap, [[b * s, a], [s, b]])

    def mmk(out, pairs, skip=False, start=True, stop=True):
        def f(E):
            last = None
            n = len(pairs)
            for i, (l, r) in enumerate(pairs):
                last = E.matmul(out, lhsT=l, rhs=r, start=(start and i == 0), stop=(stop and i == n - 1),
                                skip_group_check=skip)
            return last
        return f

    def mml(items):
        def f(E):
            last = None
            for (o, l, r, st, sp) in items:
                last = E.matmul(o, lhsT=l, rhs=r, start=st, stop=sp)
            return last
        return f

    def act(out, in_, func, **kw):
        return lambda E: E.activation(out=out, in_=in_, func=func, **kw)

    def tt(out, in0, in1, op):
        return lambda E: E.tensor_tensor(out=out, in0=in0, in1=in1, op=op)

    def ts(out, in0, s1, op0, s2=None, op1=None):
        if op1 is None:
            return lambda E: E.tensor_scalar(out=out, in0=in0, scalar1=s1, scalar2=None, op0=op0)
        return lambda E: E.tensor_scalar(out=out, in0=in0, scalar1=s1, scalar2=s2, op0=op0, op1=op1)

    def stt(out, in0, scalar, in1, op0, op1):
        return lambda E: E.scalar_tensor_tensor(out=out, in0=in0, scalar=scalar, in1=in1, op0=op0, op1=op1)

    def cp(out, in_):
        return lambda E: E.tensor_copy(out=out, in_=in_)

    def ms(out, v):
        return lambda E: E.memset(out, v)

    def dm(out, in_):
        return lambda E: E.dma_start(out=out, in_=in_)

    xh = din("xh", [TH, D])
    xpad = din("xpad", [(NCORES - 1) * T + HALO, D])
    hmask_d = din("hmask", [128, 1])
    segmask_d = din("segmask", [128, 8])
    cT_d = din("cT", [128, 8])
    w_ada = din("w_ada", [D, 6 * D])
    b_ada = din("b_ada", [1, 6 * D])
    w_in = din("w_in", [D, DIN])
    pk1024_d = din("pk1024", [128, 8, 37])
    pk3072_d = din("pk3072", [128, 24, 5])
    w_cv_out = din("w_cv_out", [D, D])
    ident_d = din("ident", [128, 128])
    tri_d = din("tri", [128, 3, 128])
    rows32_d = din("rows32", [128, 96])
    gn_d = din("gn", [128, 16])
    w_ssm_out = din("w_ssm_out", [2 * D, D])
    w_mix_out = din("w_mix_out", [D, D])
    wr_d = din("wr", [D, 72])
    br_d = din("br", [128, 72])
    w1_d = din("w1", [64, D, 512])
    w3_d = din("w3", [64, D, 512])
    w2_d = din("w2", [64, 512, D])
    fg_d = din("fg", [128, D])
    out_d = nc.dram_tensor("out", [T, D], F32, kind="ExternalOutput").ap()
    b_out = Buf("out")

    d_m1T = dscr("d_m1T", [128, 8, T], F32)
    b_dm1 = Buf("d_m1T")
    d_ynT = dscr("d_ynT", [128, 16, T], BF16)
    b_dyn = Buf("d_ynT")
    d_mT = dscr("d_mT", [128, 8, T], BF16)
    b_dmT = Buf("d_mT")
    d_h = dscr("d_h", [T, D], F32)
    b_dh = Buf("d_h")

    PB = []
    for i in range(8):
        PB.append((nc.alloc_psum_tensor("pb%d" % i, [128, 512], F32), Buf("pb%d" % i)))
    TS = [(PB[5 + s // 4][0][:, (s % 4) * 128:(s % 4 + 1) * 128], Buf("ts%d" % s)) for s in range(8)]

    def dbg_finish(items):
        outs = []
        for name, ap, shape, dt, bufs in items:
            o = nc.dram_tensor(name, list(shape), dt, kind="ExternalOutput").ap()
            bo = Buf(name)
            S.dma("sp", dm(o, ap), reads=bufs, writes=[bo])
            outs.append(bo)
        S.finish(outs)
        return nc

    L0 = Scope(nc, S)
    L0.__enter__()
    ident, b_ident = L0.sb("ident", [128, 128], F32)
    identb, b_identb = L0.sb("identb", [128, 128], BF16)
    onesb, b_onesb = L0.sb("onesb", [128, 128], BF16)
    onesf, b_onesf = L0.sb("onesf", [128, 128], F32)
    hmask, b_hmask = L0.sb("hmask", [128, 1], F32)
    pk1024, b_pk1024 = L0.sb("pk1024", [128, 8, 37], F32)
    modT, b_modT = L0.sb("modT", [128, 48], F32)
    g1bc, b_g1bc = L0.sb("g1bc", [128, D], F32)
    g2bc, b_g2bc = L0.sb("g2bc", [128, D], F32)
    s1col, b_s1col = L0.sb("s1col", [128, 8], F32)
    s2col, b_s2col = L0.sb("s2col", [128, 8], F32)
    gate, b_gate = L0.sb("gate", [128, NT, 64], F32)
    S.dma("sp", dm(ident[:], ident_d), writes=[b_ident])
    S.dma("sp", dm(hmask[:], hmask_d), writes=[b_hmask])
    S.dma("sp", dm(pk1024[:], pk1024_d), writes=[b_pk1024])
    S.op("dve", cp(identb[:], ident[:]), reads=[b_ident], writes=[b_identb])
    S.op("dve", ms(onesb[:], 1.0), writes=[b_onesb])
    S.op("dve", ms(onesf[:], 1.0), writes=[b_onesf])

    with Scope(nc, S) as sc:
        modrow, b_modrow = sc.sb("modrow", [1, 6 * D], F32)
        badar, b_badar = sc.sb("badar", [1, 6 * D], F32)
        cTs, b_cTs = sc.sb("cTs", [128, 8], F32)
        scb, b_scb = sc.sb("scb", [128, 8], BF16)
        wada = [sc.sb("wada%d" % i, [128, 8, 512], BF16) for i in range(2)]
        S.dma("sp", dm(cTs[:], cT_d), writes=[b_cTs])
        S.dma("sp", dm(badar[:], b_ada), writes=[b_badar])
        S.op("act", act(scb[:], cTs[:], AF.Silu), reads=[b_cTs], writes=[b_scb])
        for cb in range(12):
            wt, bw = wada[cb % 2]
            S.dma("pool", dm(wt[:], wview(w_ada, cb * 512, 512)), writes=[bw])
            pt_, bp = PB[cb % 2]
            S.op("pe", mmk(pt_[0:1, :], [(scb[:, k:k + 1], wt[:, k, :]) for k in range(8)]), reads=[b_scb, bw], writes=[bp])
            S.op("dve", tt(modrow[0:1, cb * 512:(cb + 1) * 512], pt_[0:1, :], badar[0:1, cb * 512:(cb + 1) * 512], ALU.add),
                 reads=[bp, b_badar], writes=[b_modrow])
        pcol, bpcol = PB[2]
        S.op("pe", mml([(pcol[:, j:j + 1], modrow[0:1, j * 128:(j + 1) * 128], onesf[0:1, 0:1], True, True)
                        for j in range(48)]), reads=[b_modrow, b_onesf], writes=[bpcol])
        S.op("dve", cp(modT[:], pcol[:, 0:48]), reads=[bpcol], writes=[b_modT])
        for (dst, bd, base) in ((g1bc, b_g1bc, 2 * D), (g2bc, b_g2bc, 5 * D)):
            for hb in range(2):
                pt_, bp = PB[3 + hb]
                S.op("pe", mmk(pt_[:], [(onesf[0:1, :], modrow[0:1, base + hb * 512: base + (hb + 1) * 512])]),
                     reads=[b_modrow, b_onesf], writes=[bp])
                S.op("act", act(dst[:, hb * 512:(hb + 1) * 512], pt_[:], AF.Copy), reads=[bp], writes=[bd])
        S.op("dve", stt(s1col[:], modT[:, 8:16], 1.0, pk1024[:, :, 0], ALU.add, ALU.mult),
             reads=[b_modT, b_pk1024], writes=[b_s1col])
        S.op("dve", stt(s2col[:], modT[:, 32:40], 1.0, pk1024[:, :, 4], ALU.add, ALU.mult),
             reads=[b_modT, b_pk1024], writes=[b_s2col])

    def stage1(sc, src, n1T, b_n1T):
        xt_bufs = [sc.sb("xt%d" % i, [128, D], F32) for i in range(2)]
        xn_bufs = [sc.sb("xn%d" % i, [128, D], BF16) for i in range(2)]
        sq_t, b_sq = sc.sb("sq", [128, D], BF16)
        st_bufs = [sc.sb("st%d" % i, [128, 2], F32) for i in range(2)]
        for i in range(NT + 1):
            rows = HALO if i == 0 else 128
            r0 = 0 if i == 0 else HALO + (i - 1) * 128
            xt, bxt = xt_bufs[i % 2]
            xn, bxn = xn_bufs[i % 2]
            st, bst = st_bufs[i % 2]
            S.dma("sp", dm(xt[0:rows, :], src[r0:r0 + rows, :]), writes=[bxt])
            S.op("act", act(sq_t[0:rows, :], xt[0:rows, :], AF.Square, scale=1.0 / 32, accum_out=st[0:rows, 0:1]),
                 reads=[bxt], writes=[b_sq, bst])
            S.op("act", act(st[0:rows, 1:2], st[0:rows, 0:1], AF.Sqrt, bias=EPS), reads=[bst], writes=[bst])
            S.op("dve", lambda E, st=st, rows=rows: E.reciprocal(out=st[0:rows, 1:2], in_=st[0:rows, 1:2]),
                 reads=[bst], writes=[bst])
            S.op("dve", ts(xn[0:rows, :], xt[0:rows, :], st[0:rows, 1:2], ALU.mult), reads=[bxt, bst], writes=[bxn])
            for k in range(8):
                tsa, ptb = TS[k]
                S.op("pe", mmk(tsa[:, 0:rows], [(xn[0:rows, k * 128:(k + 1) * 128], identb[0:rows, 0:rows])]),
                     reads=[bxn, b_identb], writes=[ptb])
                S.op("dve", ts(n1T[:, k, r0:r0 + rows], tsa[:, 0:rows], s1col[:, k:k + 1], ALU.mult,
                               modT[:, k:k + 1], ALU.add),
                     reads=[ptb, b_s1col, b_modT], writes=[b_n1T[i]])

    def n1_bufs_for(b_n1T, c0, n):
        out = []
        for i in range(NT + 1):
            lo = 0 if i == 0 else HALO + (i - 1) * 128
            hi = HALO if i == 0 else lo + 128
            if lo < c0 + n and hi > c0:
                out.append(b_n1T[i])
        return out

    TBLK = [(0, HALO)] + [(HALO + 512 * j, 512) for j in range(4)]

    LA = Scope(nc, S)
    LA.__enter__()
    n1T, _ = LA.sb("n1T", [128, 8, TH], BF16)
    b_n1T = [Buf("n1T_%d" % i) for i in range(NT + 1)]

    LS = Scope(nc, S)
    LS.__enter__()
    pk3072, b_pk3072 = LS.sb("pk3072", [128, 24, 5], F32)
    tri, b_tri = LS.sb("tri", [128, 3, 128], F32)
    Ub, b_Ub = LS.sb("Ub", [128, 128], BF16)
    Vb, b_Vb = LS.sb("Vb", [128, 128], BF16)
    NEGB, b_NEGB = LS.sb("NEGB", [128, 512], BF16)
    rows32, b_rows = LS.sb("rows32", [128, 96], F32)
    arow, b_arow = LS.sb("arow", [128, 32], F32)
    gn, b_gn = LS.sb("gn", [128, 16], F32)
    segmask, b_segmask = LS.sb("segmask", [128, 8], F32)
    wdt, b_wdt = LS.sb("wdt", [128, 8, 32], BF16)
    stf, b_stf = LS.sb("stf", [128, 2048], F32)
    stb, b_stb = LS.sb("stb", [128, 2048], BF16)
    b_stfg = [Buf("stf%d" % g) for g in range(4)]
    b_stbg = [Buf("stb%d" % g) for g in range(4)]
    S.dma("sp", dm(pk3072[:], pk3072_d), writes=[b_pk3072])
    S.dma("sp", dm(tri[:], tri_d), writes=[b_tri])
    S.dma("sp", dm(rows32[:], rows32_d), writes=[b_rows])
    S.dma("sp", dm(gn[:], gn_d), writes=[b_gn])
    S.dma("sp", dm(segmask[:], segmask_d), writes=[b_segmask])
    S.dma("pool", dm(wdt[:], wview(w_in, C_DT, 32)), writes=[b_wdt])
    S.op("dve", cp(Ub[:], tri[:, 0, :]), reads=[b_tri], writes=[b_Ub])
    S.op("dve", cp(Vb[:], tri[:, 1, :]), reads=[b_tri], writes=[b_Vb])
    S.op("dve", cp(v3(NEGB[:], 4, 128), bc_mid(tri[:, 2, :], 4)), reads=[b_tri], writes=[b_NEGB])
    S.op("act", act(arow[:], rows32[:, 32:64], AF.Exp), reads=[b_rows], writes=[b_arow])
    S.op("dve", ts(arow[:], arow[:], -1.0, ALU.mult), reads=[b_arow], writes=[b_arow])
    S.op("dve", ms(stf[:], 0.0), writes=b_stfg)
    Uf, Vf = tri[:, 0, :], tri[:, 1, :]
    dtb, dsk = rows32[:, 0:32], rows32[:, 64:96]

    def dt_pipe(sc, n1T, b_n1T, pre, maskcol=None):
        R = {}
        R["dt"], R["b_dt"] = sc.sb("dt", [128, NT, 32], F32)
        R["ad"], R["b_ad"] = sc.sb("ad", [128, NT, 32], F32)
        R["EX"], R["b_EX"] = sc.sb("EX", [128, NT, 96], F32)
        R["wgt"], R["b_wgt"] = sc.sb("wgt", [128, NT, 32], F32)
        R["Dj"], R["b_Dj"] = sc.sb("Dj", [128, 32], F32)
        t0, bt0 = sc.sb("dt_t0", [128, NT, 32], F32)
        sfx, bsfx = sc.sb("dt_sfx", [128, NT + 1, 32], F32)
        tots, btots = sc.sb("dt_tots", [128, NT, 32], F32)
        dt, ad, EX, wgt = R["dt"], R["ad"], R["EX"], R["wgt"]
        b_dt, b_ad, b_EX, b_wgt = R["b_dt"], R["b_ad"], R["b_EX"], R["b_wgt"]
        pd, bpd = PB[3]
        items = []
        for i in range(NT):
            tsl = slice(HALO + i * 128, HALO + (i + 1) * 128)
            for k in range(8):
                items.append((pd[:, i * 32:(i + 1) * 32], n1T[:, k, tsl], wdt[:, k, :], k == 0, k == 7))
        S.op("pe", mml(items), reads=[b_wdt] + b_n1T[1:], writes=[bpd])
        S.op("dve", tt(t0[:], v3(pd[:], NT, 32), bc_mid(dtb, NT), ALU.add), reads=[bpd, b_rows], writes=[bt0])
        S.op("act", act(t0[:], t0[:], AF.Exp), reads=[bt0], writes=[bt0])
        S.op("act", act(dt[:], t0[:], AF.Ln, bias=1.0), reads=[bt0], writes=[b_dt])
        if pre:
            S.op("dve", ts(dt[:], dt[:], maskcol, ALU.mult), reads=[b_dt, b_segmask], writes=[b_dt])
        S.op("dve", tt(ad[:], dt[:], bc_mid(arow[:], NT), ALU.mult), reads=[b_dt, b_arow], writes=[b_ad])
        adf = APX(ad[:], [[1, NT * 32]])
        (p4, bp4), (p5, bp5), (p6, bp6) = PB[4], PB[5], PB[6]
        S.op("pe", mml([(p4[:], Uf, adf, True, True), (p5[:], Vf, adf, True, True), (p6[:], onesf[:], adf, True, True)]),
             reads=[b_ad, b_tri, b_onesf], writes=[bp4, bp5, bp6])
        if not pre:
            S.op("act", act(EX[:, :, 0:32], v3(p4[:], NT, 32), AF.Exp), reads=[bp4], writes=[b_EX])
            S.op("act", act(EX[:, :, 32:64], v3(p5[:], NT, 32), AF.Exp), reads=[bp5], writes=[b_EX])
            S.op("act", act(EX[:, :, 64:96], v3(p6[:], NT, 32), AF.Exp), reads=[bp6], writes=[b_EX])
        else:
            S.op("dve", cp(tots[:], v3(p6[:], NT, 32)), reads=[bp6], writes=[btots])
            S.op("dve", ms(sfx[:, NT - 1:NT + 1, :], 0.0), writes=[bsfx])
            for i in range(NT - 2, -2, -1):
                dst = sfx[:, i, :] if i >= 0 else sfx[:, NT, :]
                S.op("dve", tt(dst, sfx[:, i + 1, :], tots[:, i + 1, :], ALU.add), reads=[bsfx, btots], writes=[bsfx])
            S.op("dve", tt(t0[:], v3(p4[:], NT, 32), sfx[:, 0:NT, :], ALU.add), reads=[bp4, bsfx, bt0], writes=[bt0])
            S.op("act", act(t0[:], t0[:], AF.Exp), reads=[bt0], writes=[bt0])
            S.op("dve", tt(wgt[:], t0[:], dt[:], ALU.mult), reads=[bt0, b_dt], writes=[b_wgt])
            S.op("act", act(R["Dj"][:], sfx[:, NT, :], AF.Exp), reads=[bsfx], writes=[R["b_Dj"]])
        return R

    def dt_pipe_old(sc, n1T, b_n1T, pre, maskcol=None):
        R = {}
        R["dt"], R["b_dt"] = sc.sb("dt", [128, NT, 32], F32)
        R["ad"], R["b_ad"] = sc.sb("ad", [128, NT, 32], F32)
        R["EX"], R["b_EX"] = sc.sb("EX", [128, NT, 96], F32)
        R["wgt"], R["b_wgt"] = sc.sb("wgt", [128, NT, 32], F32)
        R["run"], R["b_run"] = sc.sb("run", [128, 32], F32)
        R["Dj"], R["b_Dj"] = sc.sb("Dj", [128, 32], F32)
        t0s = [sc.sb("dt_t0_%d" % i, [128, 32], F32) for i in range(2)]
        t1s = [sc.sb("dt_t1_%d" % i, [128, 32], F32) for i in range(2)]
        dt, ad, EX, wgt, run = R["dt"], R["ad"], R["EX"], R["wgt"], R["run"]
        b_dt, b_ad, b_EX, b_wgt, b_run = R["b_dt"], R["b_ad"], R["b_EX"], R["b_wgt"], R["b_run"]
        pd, bpd = PB[6]
        pc, bpc = PB[7]
        if pre:
            S.op("dve", ms(run[:], 0.0), writes=[b_run])
        order = range(NT - 1, -1, -1) if pre else range(NT)
        for n_, i in enumerate(order):
            tsl = slice(HALO + i * 128, HALO + (i + 1) * 128)
            t0, bt0 = t0s[n_ % 2]
            t1, bt1 = t1s[n_ % 2]
            S.op("pe", mmk(pd[:, 0:32], [(n1T[:, k, tsl], wdt[:, k, :]) for k in range(8)]),
                 reads=[b_n1T[i + 1], b_wdt], writes=[bpd])
            S.op("dve", tt(t0[:], pd[:, 0:32], dtb, ALU.add), reads=[bpd, b_rows], writes=[bt0])
            S.op("act", act(t0[:], t0[:], AF.Exp), reads=[bt0], writes=[bt0])
            S.op("act", act(dt[:, i, :], t0[:], AF.Ln, bias=1.0), reads=[bt0], writes=[b_dt])
            if pre:
                S.op("dve", ts(dt[:, i, :], dt[:, i, :], maskcol, ALU.mult), reads=[b_dt, b_segmask], writes=[b_dt])
            S.op("dve", tt(ad[:, i, :], dt[:, i, :], arow[:], ALU.mult), reads=[b_dt, b_arow], writes=[b_ad])
            S.op("pe", mml([(pc[:, 0:32], Uf, ad[:, i, :], True, True),
                            (pc[:, 32:64], Vf, ad[:, i, :], True, True),
                            (pc[:, 64:96], onesf[:], ad[:, i, :], True, True)]),
                 reads=[b_ad, b_tri, b_onesf], writes=[bpc])
            if not pre:
                S.op("act", act(EX[:, i, :], pc[:, 0:96], AF.Exp), reads=[bpc], writes=[b_EX])
            else:
                S.op("dve", tt(t1[:], pc[:, 0:32], run[:], ALU.add), reads=[bpc, b_run], writes=[bt1])
                S.op("act", act(t1[:], t1[:], AF.Exp), reads=[bt1], writes=[bt1])
                S.op("dve", tt(wgt[:, i, :], t1[:], dt[:, i, :], ALU.mult), reads=[bt1, b_dt], writes=[b_wgt])
                S.op("dve", tt(run[:], run[:], pc[:, 64:96], ALU.add), reads=[bpc, b_run], writes=[b_run])
        if pre:
            S.op("act", act(R["Dj"][:], run[:], AF.Exp), reads=[b_run], writes=[R["b_Dj"]])
        return R

    def prep_group(sc, n1T, b_n1T, g, nch, halo_mode):
        chunk_ids = [g * 4 + cc for cc in range(4)] + [16 + g] + ([20 + g] if nch == 6 else [])
        wblk, b_wblk = sc.sb("wblk", [128, 8, nch * 128], BF16)
        dg, _ = sc.sb("dg4", [128, nch * 4, 128], BF16)
        b_dg = [Buf("dg4_%d" % cc) for cc in range(nch)]
        xpost, _ = sc.sb("xpost", [128, nch, T], BF16)
        b_xpost = [[Buf("xpost_%d_%d" % (cc, j)) for j in range(4)] for cc in range(nch)]
        xpre_bufs = [sc.sb("xpre%d" % i, [128, TH], BF16) for i in range(2)]
        S.dma("pool", dm(wblk[:, :, 0:512], wview(w_in, C_XBC + g * 512, 512)), writes=[b_wblk])
        S.dma("pool", dm(wblk[:, :, 512:640], wview(w_in, C_B + g * 128, 128)), writes=[b_wblk])
        if nch == 6:
            S.dma("pool", dm(wblk[:, :, 640:768], wview(w_in, C_C + g * 128, 128)), writes=[b_wblk])
        for cc in range(nch):
            for k in range(4):
                S.op("dve", ts(dg[:, cc * 4 + k, :], identb[:], pk3072[:, chunk_ids[cc], 1 + k:2 + k], ALU.mult),
                     reads=[b_identb, b_pk3072], writes=[b_dg[cc]])
        for cc in range(nch):
            xp, bxp = xpre_bufs[cc % 2]
            for jb, (c0, n) in enumerate(TBLK):
                pt_, bp = PB[jb % 2]
                if jb == 0 and halo_mode == "zero":
                    S.op("dve", ms(xp[:, 0:HALO], 0.0), writes=[bxp])
                    continue
                S.op("pe", mmk(pt_[:, 0:n], [(wblk[:, k, cc * 128:(cc + 1) * 128], n1T[:, k, c0:c0 + n]) for k in range(8)]),
                     reads=[b_wblk] + n1_bufs_for(b_n1T, c0, n), writes=[bp])
                if jb == 0 and halo_mode == "mask":
                    S.op("dve", ts(xp[:, 0:n], pt_[:, 0:n], hmask[:, 0:1], ALU.mult), reads=[bp, b_hmask], writes=[bxp])
                else:
                    S.op("dve", cp(xp[:, c0:c0 + n], pt_[:, 0:n]), reads=[bp], writes=[bxp])
            for j in range(4):
                c0 = HALO + 512 * j
                pc_, bpc = PB[2 + j % 2]
                S.op("pe", mmk(pc_[:], [(dg[:, cc * 4 + k, :], xp[:, c0 - 3 + k:c0 - 3 + k + 512]) for k in range(4)]),
                     reads=[b_dg[cc], bxp], writes=[bpc])
                S.op("act", act(xpost[:, cc, 512 * j:512 * (j + 1)], pc_[:], AF.Silu, bias=pk3072[:, chunk_ids[cc], 0:1]),
                     reads=[bpc, b_pk3072], writes=[b_xpost[cc][j]])
        return xpost, b_xpost

    b_p7h = [Buf("p7h0"), Buf("p7h1")]
    for jseg in range(npre):
        with Scope(nc, S) as sp:
            with Scope(nc, S) as s1:
                stage1(s1, xpad[jseg * T:jseg * T + TH, :], n1T, b_n1T)
            with Scope(nc, S) as s2:
                R = dt_pipe(s2, n1T, b_n1T, True, segmask[:, jseg:jseg + 1])
                xd_bufs = [s2.sb("xdp%d" % i, [128, 512], BF16) for i in range(2)]
                bt_bufs = [s2.sb("btp%d" % i, [128, 128], BF16) for i in range(2)]
                for g in range(4):
                    with Scope(nc, S) as s3:
                        xpost, b_xpost = prep_group(s3, n1T, b_n1T, g, 5, "zero" if jseg == 0 else "plain")
                        pst, bpst = PB[4]
                        for i in range(NT):
                            tsl = slice(i * 128, (i + 1) * 128)
                            px, bpx = PB[5]
                            pbt, bpbt = PB[6]
                            xd, bxd = xd_bufs[i % 2]
                            bt, bbt = bt_bufs[i % 2]
                            S.op("pe", mml([(px[:, cc * 128:(cc + 1) * 128], xpost[:, cc, tsl], identb[:], True, True)
                                            for cc in range(4)]),
                                 reads=[b_identb] + [b_xpost[cc][i // 4] for cc in range(4)], writes=[bpx])
                            S.op("pe", mmk(pbt[:, 0:128], [(xpost[:, 4, tsl], identb[:])]),
                                 reads=[b_identb, b_xpost[4][i // 4]], writes=[bpbt])
                            S.op("dve", tt(v3(xd[:], 8, 64), v3(px[:], 8, 64), bc_in(R["wgt"][:, i, g * 8:(g + 1) * 8], 64), ALU.mult),
                                 reads=[bpx, R["b_wgt"]], writes=[bxd])
                            S.op("act", act(bt[:], pbt[:, 0:128], AF.Copy), reads=[bpbt], writes=[bbt])
                            S.op("pe", mmk(pst[:], [(bt[:], xd[:])], skip=True, start=(i == 0), stop=(i == NT - 1)),
                                 reads=[bbt, bxd], writes=[bpst])
                        sl = slice(g * 512, (g + 1) * 512)
                        S.op("pool", tt(v3(stf[:, sl], 8, 64), v3(stf[:, sl], 8, 64), bc_in(R["Dj"][:, g * 8:(g + 1) * 8], 64), ALU.mult),
                             reads=[b_stfg[g], R["b_Dj"]], writes=[b_stfg[g]])
                        S.op("dve", tt(stf[:, sl], stf[:, sl], pst[:], ALU.add), reads=[b_stfg[g], bpst], writes=[b_stfg[g]])

    if stage == "PRE":
        return dbg_finish([("o_stf", stf[:], [128, 2048], F32, b_stfg)])

    with Scope(nc, S) as sc:
        stage1(sc, xh, n1T, b_n1T)

    for g in range(4):
        S.op("act", act(stb[:, g * 512:(g + 1) * 512], stf[:, g * 512:(g + 1) * 512], AF.Copy),
             reads=[b_stfg[g]], writes=[b_stbg[g]])
    with Scope(nc, S) as s2:
        R = dt_pipe(s2, n1T, b_n1T, False)
        dt, ad, EX = R["dt"], R["ad"], R["EX"]
        wz_t, b_wz = s2.sb("wz", [128, 8, 512], BF16)

        def tb(name, shape, dtp, n=2):
            return [s2.sb("%s%d" % (name, i), shape, dtp) for i in range(n)]
        xdt_b, xsD_b, xd_b = tb("xdt", [128, 512], BF16), tb("xsD", [128, 512], BF16), tb("xd", [128, 512], BF16)
        bt_b, cb_b = tb("btk", [128, 128], BF16), tb("cbs", [128, 128], BF16)
        adV_b, LT_b, MT_b = tb("adV", [128, 1024], BF16), tb("LT", [128, 1024], BF16), tb("MT", [128, 1024], BF16)
        t1_b, ys_b, zs_b, yz_b = tb("t1", [128, 512], F32), tb("ysb", [128, 512], F32), tb("zs", [128, 512], F32), tb("yz", [128, 512], F32)
        sq_b, yn_b = tb("sqj", [128, 512], BF16), tb("yn", [128, 512], BF16)
        ss_b = tb("ss", [128, 2], F32)
        ynT_b = tb("ynTt", [128, 4, 128], BF16)
        it = 0
        pending = []
        for g in range(4):
            g8 = slice(g * 8, (g + 1) * 8)
            gsl = slice(g * 512, (g + 1) * 512)
            with Scope(nc, S) as s3:
                xpost, b_xpost = prep_group(s3, n1T, b_n1T, g, 6, "mask")
                S.dma("pool", dm(wz_t[:], wview(w_in, C_Z + g * 512, 512)), writes=[b_wz])
                for i in range(NT):
                    q = it % 2
                    it += 1
                    tsl = slice(i * 128, (i + 1) * 128)
                    hsl = slice(HALO + i * 128, HALO + (i + 1) * 128)
                    rx = [b_xpost[cc][i // 4] for cc in range(4)]
                    rB, rC = b_xpost[4][i // 4], b_xpost[5][i // 4]
                    (xdt, bxdt), (xsD, bxsD), (xd, bxd) = xdt_b[q], xsD_b[q], xd_b[q]
                    (bt, bbt), (cbs, bcbs) = bt_b[q], cb_b[q]
                    (adV, badV), (LT, bLT), (MT, bMT) = adV_b[q], LT_b[q], MT_b[q]
                    (t1, bt1), (ysb, bys), (zs, bzs), (yz, byz) = t1_b[q], ys_b[q], zs_b[q], yz_b[q]
                    (sqj, bsq), (yn, byn), (ss, bss), (ynTt, bynT) = sq_b[q], yn_b[q], ss_b[q], ynT_b[q]
                    px, bpx = PB[2]
                    p3, bp3 = PB[3]
                    S.op("pe", mml([(px[:, cc * 128:(cc + 1) * 128], xpost[:, cc, tsl], identb[:], True, True) for cc in range(4)]),
                         reads=[b_identb] + rx, writes=[bpx])
                    S.op("pe", mml([(p3[:, 0:128], xpost[:, 4, tsl], identb[:], True, True),
                                    (p3[:, 128:256], xpost[:, 4, tsl], xpost[:, 5, tsl], True, True)]),
                         reads=[b_identb, rB, rC], writes=[bp3])
                    S.op("dve", tt(v3(xdt[:], 8, 64), v3(px[:], 8, 64), bc_in(dt[:, i, g8], 64), ALU.mult),
                         reads=[bpx, R["b_dt"]], writes=[bxdt])
                    S.op("dve", tt(v3(xsD[:], 8, 64), v3(px[:], 8, 64), bc_in(rows32[:, 64 + g * 8:72 + g * 8], 64), ALU.mult),
                         reads=[bpx, b_rows], writes=[bxsD])
                    S.op("pool", tt(v3(xd[:], 8, 64), v3(xdt[:], 8, 64), bc_in(EX[:, i, g * 8:(g + 1) * 8], 64), ALU.mult),
                         reads=[bxdt, R["b_EX"]], writes=[bxd])
                    S.op("act", act(bt[:], p3[:, 0:128], AF.Copy), reads=[bp3], writes=[bbt])
                    S.op("act", act(cbs[:], p3[:, 128:256], AF.Copy), reads=[bp3], writes=[bcbs])
                    S.op("dve", tt(v3(adV[:], 8, 128), bc_mid(Vb[:], 8), bc_in(ad[:, i, g8], 128), ALU.mult),
                         reads=[b_Vb, R["b_ad"]], writes=[badV])
                    for half in range(2):
                        ph, bph = PB[half]
                        S.op("pe", mml([(ph[:], Ub[:], adV[:, half * 512:(half + 1) * 512], True, False),
                                        (ph[:], identb[:], NEGB[:], False, True)]),
                             reads=[b_Ub, badV, b_identb, b_NEGB], writes=[bph])
                        S.op("act", act(LT[:, half * 512:(half + 1) * 512], ph[:], AF.Exp), reads=[bph], writes=[bLT])
                    S.op("dve", tt(v3(MT[:], 8, 128), v3(LT[:], 8, 128), bc_mid(cbs[:], 8), ALU.mult),
                         reads=[bLT, bcbs], writes=[bMT])
                    py, bpy = PB[4]
                    po, bpo = PB[5]
                    pst, bpst = PB[6]
                    items = []
                    for r in range(8):
                        rs = slice(r * 64, (r + 1) * 64)
                        items.append((py[:, rs], identb[:], xsD[:, rs], True, False))
                        items.append((py[:, rs], MT[:, r * 128:(r + 1) * 128], xdt[:, rs], False, True))
                    S.op("pe", mml(items), reads=[b_identb, bxsD, bMT, bxdt], writes=[bpy])
                    S.op("pe", mmk(po[:], [(xpost[:, 5, tsl], stb[:, gsl])]), reads=[rC, b_stbg[g]], writes=[bpo])
                    S.op("pe", mmk(pst[:], [(bt[:], xd[:])]), reads=[bbt, bxd], writes=[bpst])
                    S.op("dve", tt(v3(t1[:], 8, 64), v3(po[:], 8, 64), bc_in(EX[:, i, 32 + g * 8:40 + g * 8], 64), ALU.mult),
                         reads=[bpo, R["b_EX"]], writes=[bt1])
                    S.op("dve", tt(ysb[:], py[:], t1[:], ALU.add), reads=[bpy, bt1], writes=[bys])
                    S.op("pool", tt(v3(stf[:, gsl], 8, 64), v3(stf[:, gsl], 8, 64), bc_in(EX[:, i, 64 + g * 8:72 + g * 8], 64), ALU.mult),
                         reads=[b_stfg[g], R["b_EX"]], writes=[b_stfg[g]])
                    S.op("dve", tt(stf[:, gsl], stf[:, gsl], pst[:], ALU.add), reads=[b_stfg[g], bpst], writes=[b_stfg[g]])
                    S.op("pool", cp(stb[:, gsl], stf[:, gsl]), reads=[b_stfg[g]], writes=[b_stbg[g]])
                    pz, bpz = PB[7]
                    S.op("pe", mmk(pz[:], [(n1T[:, k, hsl], wz_t[:, k, :]) for k in range(8)]),
                         reads=[b_n1T[i + 1], b_wz], writes=[bpz])
                    S.op("act", act(zs[:], pz[:], AF.Silu), reads=[bpz], writes=[bzs])
                    S.op("dve", tt(yz[:], ysb[:], zs[:], ALU.mult), reads=[bys, bzs], writes=[byz])
                    S.op("act", act(sqj[:], yz[:], AF.Square, scale=float(512 ** -0.5), accum_out=ss[:, 0:1]),
                         reads=[byz], writes=[bsq, bss])
                    S.op("act", act(ss[:, 1:2], ss[:, 0:1], AF.Sqrt, bias=EPS), reads=[bss], writes=[bss])
                    S.op("dve", lambda E, ss=ss: E.reciprocal(out=ss[:, 1:2], in_=ss[:, 1:2]), reads=[bss], writes=[bss])
                    S.op("dve", ts(yn[:], yz[:], ss[:, 1:2], ALU.mult), reads=[byz, bss], writes=[byn])

                    def tail(g=g, tsl=tsl, yn=yn, byn=byn, ynTt=ynTt, bynT=bynT):
                        pt_, bpt_ = PB[2]
                        S.op("pe", mml([(pt_[:, cc * 128:(cc + 1) * 128], yn[:, cc * 128:(cc + 1) * 128], identb[:], True, True)
                                        for cc in range(4)]), reads=[byn, b_identb], writes=[bpt_])
                        S.op("dve", tt(ynTt[:], v3(pt_[:], 4, 128), bc_in(gn[:, g * 4:(g + 1) * 4], 128), ALU.mult),
                             reads=[bpt_, b_gn], writes=[bynT])
                        S.dma("sp", dm(d_ynT[:, g * 4:(g + 1) * 4, tsl], ynTt[:]), reads=[bynT], writes=[b_dyn])
                    tail()
                    if i == NT - 1:
                        while len(pending) > 0:
                            pending.pop(0)()
                    if stage == "B1" and g == 0 and i == 1:
                        return dbg_finish([("o_ys", ysb[:], [128, 512], F32, [bys]),
                                           ("o_yn", yn[:], [128, 512], BF16, [byn])])
    LS.__exit__(None, None, None)
    if stage == "B":
        with Scope(nc, S) as sc:
            yt, byt = sc.sb("ynall", [128, 16, T], BF16)
            S.dma("sp", dm(yt[:], d_ynT), reads=[b_dyn], writes=[byt])
            return dbg_finish([("o_ynT", yt[:], [128, 16, T], BF16, [byt])])

    LB = Scope(nc, S)
    LB.__enter__()
    cuT, _ = LB.sb("cuT", [128, 8, T], BF16)
    b_cuT = [[Buf("cuT_%d_%d" % (c, j)) for j in range(4)] for c in range(8)]
    with Scope(nc, S) as scu:
        uT, _ = scu.sb("uT", [128, 8, TH], BF16)
        b_uT = [[Buf("uT_%d_%d" % (c, j)) for j in range(5)] for c in range(8)]
        with Scope(nc, S) as sc:
            wg_bufs = [sc.sb("wg%d" % i, [128, 8, 512], BF16) for i in range(4)]
            sg_bufs = [sc.sb("sg%d" % i, [128, 512], F32) for i in range(2)]
            ecnt = 0
            for blk in range(2):
                wa, bwa = wg_bufs[(2 * blk) % 4]
                wb, bwb = wg_bufs[(2 * blk + 1) % 4]
                S.dma("pool", dm(wa[:], wview(w_in, C_GLU_A + blk * 512, 512)), writes=[bwa])
                S.dma("pool", dm(wb[:], wview(w_in, C_GLU_B + blk * 512, 512)), writes=[bwb])
                for cc in range(4):
                    c = blk * 4 + cc
                    for j, (c0, n) in enumerate(TBLK):
                        pa, bpa = PB[(2 * ecnt) % 4]
                        pb_, bpb = PB[(2 * ecnt + 1) % 4]
                        sg, bsg = sg_bufs[ecnt % 2]
                        ecnt += 1
                        rb = n1_bufs_for(b_n1T, c0, n)
                        S.op("pe", mml([(pa[:, 0:n], wa[:, k, cc * 128:(cc + 1) * 128], n1T[:, k, c0:c0 + n], k == 0, k == 7)
                                        for k in range(8)] +
                                       [(pb_[:, 0:n], wb[:, k, cc * 128:(cc + 1) * 128], n1T[:, k, c0:c0 + n], k == 0, k == 7)
                                        for k in range(8)]), reads=[bwa, bwb] + rb, writes=[bpa, bpb])
                        S.op("act", act(sg[:, 0:n], pb_[:, 0:n], AF.Sigmoid), reads=[bpb], writes=[bsg])
                        if j == 0:
                            S.op("dve", stt(uT[:, c, c0:c0 + n], pa[:, 0:n], hmask[:, 0:1], sg[:, 0:n], ALU.mult, ALU.mult),
                                 reads=[bpa, bsg, b_hmask], writes=[b_uT[c][j]])
                        else:
                            S.op("dve", tt(uT[:, c, c0:c0 + n], pa[:, 0:n], sg[:, 0:n], ALU.mult),
                                 reads=[bpa, bsg], writes=[b_uT[c][j]])
        with Scope(nc, S) as sc:
            dg_bufs = [sc.sb("dg%d" % i, [128, 31, 128], BF16) for i in range(2)]
            csq_bufs = [sc.sb("csq%d" % i, [128, 512], BF16) for i in range(2)]
            ssum, b_ssum = sc.sb("ssum", [128, T], F32)
            ssq, b_ssq = sc.sb("ssq", [128, T], F32)
            tmp_bufs = [sc.sb("lnt%d" % i, [128, 512], F32) for i in range(2)]
            for c in range(8):
                dg, b_dg = dg_bufs[c % 2]
                for k in range(31):
                    eng = "dve" if (k % 2 == 0) else "pool"
                    S.op(eng, ts(dg[:, k, :], identb[:], pk1024[:, c, 6 + k:7 + k], ALU.mult),
                         reads=[b_identb, b_pk1024], writes=[b_dg])
                for j in range(4):
                    c0 = HALO + 512 * j
                    pc, bpc = PB[j % 2]
                    ps1, bps1 = PB[2] if j % 2 == 0 else PB[7]
                    ps2, bps2 = PB[3] if j % 2 == 0 else PB[4]
                    csq, bcsq = csq_bufs[j % 2]
                    S.op("pe", mmk(pc[:], [(dg[:, k, :], uT[:, c, c0 - 30 + k: c0 - 30 + k + 512]) for k in range(31)]),
                         reads=[b_dg, b_uT[c][j + 1], b_uT[c][j]], writes=[bpc])
                    S.op("act", act(cuT[:, c, 512 * j:512 * (j + 1)], pc[:], AF.Identity, bias=pk1024[:, c, 1:2]),
                         reads=[bpc, b_pk1024], writes=[b_cuT[c][j]])
                    S.op("act", act(csq[:], pc[:], AF.Square, bias=pk1024[:, c, 1:2]), reads=[bpc, b_pk1024], writes=[bcsq])
                    S.op("pe", mmk(ps1[:], [(onesb[:], cuT[:, c, 512 * j:512 * (j + 1)])]), reads=[b_onesb, b_cuT[c][j]], writes=[bps1])
                    S.op("pe", mmk(ps2[:], [(onesb[:], csq[:])]), reads=[b_onesb, bcsq], writes=[bps2])
                    sl = slice(512 * j, 512 * (j + 1))
                    if c == 0:
                        S.op("dve", cp(ssum[:, sl], ps1[:]), reads=[bps1], writes=[b_ssum])
                        S.op("dve", cp(ssq[:, sl], ps2[:]), reads=[bps2], writes=[b_ssq])
                    else:
                        S.op("dve", tt(ssum[:, sl], ps1[:], ssum[:, sl], ALU.add), reads=[bps1, b_ssum], writes=[b_ssum])
                        S.op("dve", tt(ssq[:, sl], ps2[:], ssq[:, sl], ALU.add), reads=[bps2, b_ssq], writes=[b_ssq])
            S.op("act", act(ssum[:], ssum[:], AF.Copy, scale=1.0 / D), reads=[b_ssum], writes=[b_ssum])
            for j in range(4):
                t0, bt0 = tmp_bufs[j % 2]
                sl = slice(512 * j, 512 * (j + 1))
                S.op("dve", tt(t0[:], ssum[:, sl], ssum[:, sl], ALU.mult), reads=[b_ssum], writes=[bt0])
                S.op("dve", stt(t0[:], ssq[:, sl], 1.0 / D, t0[:], ALU.mult, ALU.subtract), reads=[b_ssq, bt0], writes=[bt0])
                S.op("act", act(ssq[:, sl], t0[:], AF.Sqrt, bias=EPS), reads=[bt0, b_ssq], writes=[b_ssq])
            S.op("dve", lambda E: E.reciprocal(out=ssq[:], in_=ssq[:]), reads=[b_ssq], writes=[b_ssq])
            cnt = 0
            for c in range(8):
                for j in range(4):
                    t1, bt1 = tmp_bufs[cnt % 2]
                    cnt += 1
                    sl = slice(512 * j, 512 * (j + 1))
                    S.op("dve", tt(t1[:], cuT[:, c, sl], ssum[:, sl], ALU.subtract), reads=[b_cuT[c][j], b_ssum], writes=[bt1])
                    S.op("pool", tt(t1[:], t1[:], ssq[:, sl], ALU.mult), reads=[bt1, b_ssq], writes=[bt1])
                    S.op("act", act(cuT[:, c, sl], t1[:], AF.Silu, scale=pk1024[:, c, 2:3], bias=pk1024[:, c, 3:4]),
                         reads=[bt1, b_pk1024], writes=[b_cuT[c][j]])
    with Scope(nc, S) as sc:
        wcv, b_wcv = sc.sb("wcv", [128, 8, D], BF16)
        wgc, b_wgc = sc.sb("wgc", [128, 8, D], BF16)
        S.dma("pool", dm(wcv[:], wview(w_cv_out, 0, D)), writes=[b_wcv])
        S.dma("pool", dm(wgc[:], wview(w_in, C_GCV, D)), writes=[b_wgc])
        m1_bufs = [sc.sb("m1s%d" % i, [128, 512], F32) for i in range(2)]
        sgt_bufs = [sc.sb("sgt%d" % i, [128, 512], F32) for i in range(2)]
        cnt = 0
        for c in range(8):
            for j in range(4):
                q = cnt % 2
                cnt += 1
                py, bpy = PB[2 * q]
                pg, bpg = PB[2 * q + 1]
                m1s, bm1s = m1_bufs[q]
                sgt, bsgt = sgt_bufs[q]
                sl = slice(512 * j, 512 * (j + 1))
                hs = slice(HALO + 512 * j, HALO + 512 * (j + 1))
                S.op("pe", mml([(py[:], wcv[:, k, c * 128:(c + 1) * 128], cuT[:, k, sl], k == 0, k == 7) for k in range(8)] +
                               [(pg[:], wgc[:, k, c * 128:(c + 1) * 128], n1T[:, k, hs], k == 0, k == 7) for k in range(8)]),
                     reads=[b_wcv, b_wgc] + n1_bufs_for(b_n1T, HALO + 512 * j, 512) + [b_cuT[k][j] for k in range(8)],
                     writes=[bpy, bpg])
                S.op("act", act(sgt[:], pg[:], AF.Sigmoid), reads=[bpg], writes=[bsgt])
                S.op("dve", tt(m1s[:], py[:], sgt[:], ALU.mult), reads=[bpy, bsgt], writes=[bm1s])
                S.dma("sp", dm(d_m1T[:, c, sl], m1s[:]), reads=[bm1s], writes=[b_dm1])
    LB.__exit__(None, None, None)
    if stage == "A":
        with Scope(nc, S) as sc:
            mt_, bmt_ = sc.sb("m1dbg", [128, 8, 512], F32)
            S.dma("sp", dm(mt_[:], d_m1T[:, :, 0:512]), reads=[b_dm1], writes=[bmt_])
            return dbg_finish([("o_m1T", mt_[:], [128, 8, 512], F32, [bmt_])])

    with Scope(nc, S) as sc:
        wso, b_wso = sc.sb("wso", [128, 16, D], BF16)
        wgs, b_wgs = sc.sb("wgs", [128, 8, D], BF16)
        S.dma("pool", dm(wso[:, 0:8, :], w_ssm_out[0:1024, :].rearrange("(k p) n -> p k n", p=128)), writes=[b_wso])
        S.dma("pool", dm(wso[:, 8:16, :], w_ssm_out[1024:2048, :].rearrange("(k p) n -> p k n", p=128)), writes=[b_wso])
        S.dma("pool", dm(wgs[:], wview(w_in, C_GSSM, D)), writes=[b_wgs])
        ynb_bufs = [sc.sb("ynb%d" % i, [128, 16, 512], BF16) for i in range(2)]
        sg_bufs = [sc.sb("sgc%d" % i, [128, 512], F32) for i in range(2)]
        mm_bufs = [sc.sb("mmc%d" % i, [128, 512], F32) for i in range(2)]
        ml_bufs = [sc.sb("mlc%d" % i, [128, 512], F32) for i in range(2)]
        mb_bufs = [sc.sb("mbc%d" % i, [128, 512], BF16) for i in range(2)]
        cnt = 0
        for j in range(4):
            ynb, bynb = ynb_bufs[j % 2]
            sl = slice(512 * j, 512 * (j + 1))
            hs = slice(HALO + 512 * j, HALO + 512 * (j + 1))
            S.dma("act", dm(ynb[:], d_ynT[:, :, sl]), reads=[b_dyn], writes=[bynb])
            for c in range(8):
                q = cnt % 2
                cnt += 1
                py, bpy = PB[2 * q]
                pg, bpg = PB[2 * q + 1]
                sg, bsg = sg_bufs[q]
                mm_, bmm = mm_bufs[q]
                ml, bml = ml_bufs[q]
                mb, bmb = mb_bufs[q]
                S.dma("sp", dm(ml[:], d_m1T[:, c, sl]), reads=[b_dm1], writes=[bml])
                S.op("pe", mml([(py[:], wso[:, k, c * 128:(c + 1) * 128], ynb[:, k, :], k == 0, k == 15) for k in range(16)] +
                               [(pg[:], wgs[:, k, c * 128:(c + 1) * 128], n1T[:, k, hs], k == 0, k == 7) for k in range(8)]),
                     reads=[b_wso, b_wgs, bynb] + n1_bufs_for(b_n1T, HALO + 512 * j, 512), writes=[bpy, bpg])
                S.op("act", act(sg[:], pg[:], AF.Sigmoid), reads=[bpg], writes=[bsg])
                S.op("dve", tt(mm_[:], py[:], sg[:], ALU.mult), reads=[bpy, bsg], writes=[bmm])
                S.op("pool", tt(mb[:], mm_[:], ml[:], ALU.add), reads=[bmm, bml], writes=[bmb])
                S.dma("sp", dm(d_mT[:, c, sl], mb[:]), reads=[bmb], writes=[b_dmT])
    if stage == "C1":
        with Scope(nc, S) as sc:
            mt_, bmt_ = sc.sb("mTdbg", [128, 8, 512], BF16)
            S.dma("sp", dm(mt_[:], d_mT[:, :, 0:512]), reads=[b_dmT], writes=[bmt_])
            return dbg_finish([("o_mT", mt_[:], [128, 8, 512], BF16, [bmt_])])
    LA.__exit__(None, None, None)

    LD = Scope(nc, S)
    LD.__enter__()
    n2T, _ = LD.sb("n2T", [128, 8, T], BF16)
    b_n2T = [Buf("n2T_%d" % i) for i in range(NT)]
    with Scope(nc, S) as sc:
        wmix, b_wmix = sc.sb("wmix", [128, 8, D], BF16)
        wr, b_wr = sc.sb("wr", [128, 8, 72], BF16)
        br, b_br = sc.sb("br", [128, 72], F32)
        S.dma("pool", dm(wmix[:], wview(w_mix_out, 0, D)), writes=[b_wmix])
        S.dma("pool", dm(wr[:], wr_d.rearrange("(k p) n -> p k n", p=128)), writes=[b_wr])
        S.dma("sp", dm(br[:], br_d), writes=[b_br])

        def tb2(name, shape, dtp, n=2):
            return [sc.sb("%s%d" % (name, i), shape, dtp) for i in range(n)]
        mt_b, xt_b, tm_b, ht_b, xn_b = tb2("mTt", [128, 8, 128], BF16), tb2("xtc", [128, D], F32), tb2("tmc", [128, D], F32), \
            tb2("htc", [128, D], F32), tb2("xnc", [128, D], F32)
        sq_t, b_sq = sc.sb("sqc", [128, D], BF16)
        st_b = tb2("stc", [128, 2], F32)
        n2f_b = tb2("n2f", [128, 8, 128], F32)
        xhi_b, xlo_b = tb2("xhi", [128, D], BF16), tb2("xlo", [128, D], BF16)
        rt_b = tb2("rt", [128, 512], F32, n=1)
        for i in range(NT):
            q = i % 2
            tsl = slice(i * 128, (i + 1) * 128)
            (mTt, bmTt), (xt, bxt), (tm, btm), (ht, bht), (xn, bxn) = mt_b[q], xt_b[q], tm_b[q], ht_b[q], xn_b[q]
            (st, bst), (n2f, bn2f) = st_b[q], n2f_b[q]
            rt, brt = rt_b[0]
            S.dma("act", dm(mTt[:], d_mT[:, :, tsl]), reads=[b_dmT], writes=[bmTt])
            S.dma("sp", dm(xt[:], xh[HALO + i * 128:HALO + (i + 1) * 128, :]), writes=[bxt])
            for ob in range(2):
                pm, bpm = PB[ob]
                osl = slice(ob * 512, (ob + 1) * 512)
                S.op("pe", mmk(pm[:], [(mTt[:, k, :], wmix[:, k, osl]) for k in range(8)]), reads=[bmTt, b_wmix], writes=[bpm])
                S.op("dve", tt(tm[:, osl], pm[:], g1bc[:, osl], ALU.mult), reads=[bpm, b_g1bc], writes=[btm])
            S.op("pool", tt(ht[:], tm[:], xt[:], ALU.add), reads=[btm, bxt], writes=[bht])
            S.dma("sp", dm(d_h[tsl, :], ht[:]), reads=[bht], writes=[b_dh])
            if stage == "C2h":
                return dbg_finish([("o_ht", ht[:], [128, D], F32, [bht, b_dh])])
            S.op("act", act(sq_t[:], ht[:], AF.Square, scale=1.0 / 32, accum_out=st[:, 0:1]), reads=[bht], writes=[b_sq, bst])
            S.op("act", act(st[:, 1:2], st[:, 0:1], AF.Sqrt, bias=EPS), reads=[bst], writes=[bst])
            S.op("dve", lambda E, st=st: E.reciprocal(out=st[:, 1:2], in_=st[:, 1:2]), reads=[bst], writes=[bst])
            S.op("dve", ts(xn[:], ht[:], st[:, 1:2], ALU.mult), reads=[bht, bst], writes=[bxn])
            (xhi, bxhi), (xlo, bxlo) = xhi_b[q], xlo_b[q]
            S.op("dve", cp(xhi[:], xn[:]), reads=[bxn], writes=[bxhi])
            for k in range(8):
                tsa, ptb = TS[k]
                S.op("pe", mmk(tsa[:, 0:128], [(xhi[:, k * 128:(k + 1) * 128], identb[:])]), reads=[bxhi, b_identb], writes=[ptb])
                S.op("dve", ts(n2T[:, k, tsl], tsa[:, 0:128], s2col[:, k:k + 1], ALU.mult, modT[:, 24 + k:25 + k], ALU.add),
                     reads=[ptb, b_s2col, b_modT], writes=[b_n2T[i]])
            if stage == "C2a" and i == 0:
                return dbg_finish([("o_ht", ht[:], [128, D], F32, [bht]), ("o_n2", n2T[:, 0, 0:128], [128, 128], BF16, [b_n2T[0]])])
            pr, bpr = PB[2]
            S.op("pe", mmk(pr[:, 0:72], [(n2T[:, k, tsl], wr[:, k, :]) for k in range(8)]), reads=[b_n2T[i], b_wr], writes=[bpr])
            lg, gmx, ohg, eg, pen = rt[:, 0:72], rt[:, 72:73], rt[:, 80:88], rt[:, 88:96], rt[:, 96:104]
            gsum, ngm, m1_, m2_, dd = rt[:, 73:74], rt[:, 74:75], rt[:, 75:76], rt[:, 76:77], rt[:, 77:78]
            w1_, w2_ = rt[:, 78:79], rt[:, 79:80]
            elm, oh1, elm2, oh2, tg = rt[:, 128:192], rt[:, 192:256], rt[:, 256:320], rt[:, 320:384], rt[:, 384:448]
            rw = dict(reads=[brt], writes=[brt])
            S.op("dve", tt(lg, pr[:, 0:72], br[:], ALU.add), reads=[bpr, b_br], writes=[brt])
            S.op("dve", lambda E, gmx=gmx, lg=lg: E.tensor_reduce(out=gmx, in_=lg[:, 0:8], axis=AX.X, op=ALU.max), **rw)
            S.op("dve", ts(ohg, lg[:, 0:8], gmx, ALU.is_equal), **rw)
            S.op("dve", ts(ngm, gmx, -1.0, ALU.mult), **rw)
            S.op("act", act(eg, lg[:, 0:8], AF.Exp, bias=ngm, accum_out=gsum), **rw)
            S.op("dve", lambda E, gsum=gsum: E.reciprocal(out=gsum, in_=gsum), **rw)
            S.op("dve", ts(pen, ohg, BIG, ALU.mult, -BIG, ALU.add), **rw)
            S.op("dve", tt(v3(elm, 8, 8), v3(lg[:, 8:72], 8, 8), bc_in(pen, 8), ALU.add), **rw)
            S.op("dve", lambda E, m1_=m1_, elm=elm: E.tensor_reduce(out=m1_, in_=elm, axis=AX.X, op=ALU.max), **rw)
            S.op("dve", ts(oh1, elm, m1_, ALU.is_equal), **rw)
            S.op("dve", stt(elm2, oh1, -BIG, elm, ALU.mult, ALU.add), **rw)
            S.op("dve", lambda E, m2_=m2_, elm2=elm2: E.tensor_reduce(out=m2_, in_=elm2, axis=AX.X, op=ALU.max), **rw)
            S.op("dve", ts(oh2, elm2, m2_, ALU.is_equal), **rw)
            S.op("dve", tt(dd, m2_, m1_, ALU.subtract), **rw)
            S.op("act", act(dd, dd, AF.Exp), **rw)
            S.op("dve", ts(w1_, dd, 1.0, ALU.add), **rw)
            S.op("dve", lambda E, w1_=w1_: E.reciprocal(out=w1_, in_=w1_), **rw)
            S.op("dve", tt(w2_, dd, w1_, ALU.mult), **rw)
            S.op("dve", ts(tg, oh1, w1_, ALU.mult), **rw)
            S.op("dve", stt(tg, oh2, w2_, tg, ALU.mult, ALU.add), **rw)
            S.op("dve", ts(gate[:, i, :], tg, gsum, ALU.mult), reads=[brt], writes=[b_gate])
            if stage == "C2b" and i == 0:
                return dbg_finish([("o_rt", rt[:], [128, 512], F32, [brt, b_gate])])
    if stage == "C":
        with Scope(nc, S) as sc:
            hh, bhh = sc.sb("hall", [128, NT, D], F32)
            S.dma("sp", dm(hh[:], d_h.rearrange("(i p) d -> p i d", p=128)), reads=[b_dh], writes=[bhh])
            return dbg_finish([("o_h", hh[:], [128, NT, D], F32, [bhh]),
                               ("o_n2T", n2T[:], [128, 8, T], BF16, b_n2T),
                               ("o_gate", gate[:], [128, NT, 64], F32, [b_gate])])

    acc, _ = LD.sb("acc", [128, NT, D], F32)
    b_acc = [Buf("acc%d" % i) for i in range(NT)]
    with Scope(nc, S) as sc:
        w13_b = [sc.sb("w13_%d" % i, [128, 8, 1024], BF16) for i in range(2)]
        w2_b = [sc.sb("w2_%d" % i, [128, 4, 1024], BF16) for i in range(2)]
        hd_b = [sc.sb("hd%d" % i, [128, 4, 512], BF16) for i in range(2)]
        s1_b = [sc.sb("s1m%d" % i, [128, 512], F32) for i in range(2)]
        cnt = 0
        hcnt = 0
        ocnt = 0
        for e in range(nexp):
            w13, bw13 = w13_b[e % 2]
            w2b, bw2 = w2_b[e % 2]
            S.dma("pool", dm(w13[:, :, 0:512], w1_d[e].rearrange("(k p) n -> p k n", p=128)), writes=[bw13])
            S.dma(MOEQ[1], dm(w13[:, :, 512:1024], w3_d[e].rearrange("(k p) n -> p k n", p=128)), writes=[bw13])
            S.dma(MOEQ[2], dm(w2b[:], w2_d[e].rearrange("(k p) n -> p k n", p=128)), writes=[bw2])
            for j in range(4):
                hd, bhd = hd_b[hcnt % 2]
                hcnt += 1
                sl = slice(512 * j, 512 * (j + 1))
                rn = [b_n2T[4 * j + t_] for t_ in range(4)]
                for fc in range(4):
                    q = cnt % 2
                    cnt += 1
                    p1, bp1 = PB[2 * q]
                    p3, bp3 = PB[2 * q + 1]
                    s1m, bs1 = s1_b[q]
                    S.op("pe", mml([(p1[:], w13[:, k, fc * 128:(fc + 1) * 128], n2T[:, k, sl], k == 0, k == 7) for k in range(8)] +
                                   [(p3[:], w13[:, k, 512 + fc * 128:512 + (fc + 1) * 128], n2T[:, k, sl], k == 0, k == 7)
                                    for k in range(8)]), reads=[bw13] + rn, writes=[bp1, bp3])
                    S.op("act", act(s1m[:], p1[:], AF.Silu), reads=[bp1], writes=[bs1])
                    S.op("dve", tt(hd[:, fc, :], s1m[:], p3[:], ALU.mult), reads=[bs1, bp3], writes=[bhd])
                for t_ in range(4):
                    i = 4 * j + t_
                    for ob in range(2):
                        po, bpo = PB[4 + ocnt % 4]
                        ocnt += 1
                        osl = slice(ob * 512, (ob + 1) * 512)
                        S.op("pe", mmk(po[:], [(hd[:, fc, t_ * 128:(t_ + 1) * 128], w2b[:, fc, osl]) for fc in range(4)]),
                             reads=[bhd, bw2], writes=[bpo])
                        if e == 0:
                            S.op("dve", ts(acc[:, i, osl], po[:], gate[:, i, e:e + 1], ALU.mult),
                                 reads=[bpo, b_gate], writes=[b_acc[i]])
                        else:
                            S.op("dve", stt(acc[:, i, osl], po[:], gate[:, i, e:e + 1], acc[:, i, osl], ALU.mult, ALU.add),
                                 reads=[bpo, b_gate, b_acc[i]], writes=[b_acc[i]])

    with Scope(nc, S) as sc:
        fg, b_fg = sc.sb("fg", [128, D], F32)
        S.dma("sp", dm(fg[:], fg_d), writes=[b_fg])
        ht_b = [sc.sb("hte%d" % i, [128, D], F32) for i in range(2)]
        o_b = [sc.sb("oe%d" % i, [128, D], F32) for i in range(2)]
        o2_b = [sc.sb("o2e%d" % i, [128, D], F32) for i in range(2)]
        sq_t, b_sq = sc.sb("sqe", [128, D], BF16)
        st_b = [sc.sb("ste%d" % i, [128, 2], F32) for i in range(2)]
        for i in range(NT):
            q = i % 2
            (ht, bht), (o, bo), (o2, bo2), (st, bst) = ht_b[q], o_b[q], o2_b[q], st_b[q]
            tsl = slice(i * 128, (i + 1) * 128)
            S.dma("sp", dm(ht[:], d_h[tsl, :]), reads=[b_dh], writes=[bht])
            S.op("dve", tt(o[:], acc[:, i, :], g2bc[:], ALU.mult), reads=[b_acc[i], b_g2bc], writes=[bo])
            S.op("pool", tt(o[:], o[:], ht[:], ALU.add), reads=[bo, bht], writes=[bo])
            S.op("act", act(sq_t[:], o[:], AF.Square, scale=1.0 / 32, accum_out=st[:, 0:1]), reads=[bo], writes=[b_sq, bst])
            S.op("act", act(st[:, 1:2], st[:, 0:1], AF.Sqrt, bias=EPS), reads=[bst], writes=[bst])
            S.op("dve", lambda E, st=st: E.reciprocal(out=st[:, 1:2], in_=st[:, 1:2]), reads=[bst], writes=[bst])
            S.op("dve", stt(o2[:], o[:], st[:, 1:2], fg[:], ALU.mult, ALU.mult), reads=[bo, bst, b_fg], writes=[bo2])
            S.dma("sp", dm(out_d[tsl, :], o2[:]), reads=[bo2], writes=[b_out])
    S.finish([b_out])
    return nc


def prep_inputs(inp):
    f = {k: np.asarray(v, dtype=np.float32) for k, v in inp.items()}
    x = f["x"][0]

    def fm(v, nch):
        v = np.atleast_2d(v)
        return np.ascontiguousarray(v.reshape(v.shape[0], nch, 128).transpose(2, 1, 0))

    def rep(v):
        return np.ascontiguousarray(np.broadcast_to(np.asarray(v, np.float32)[None, :], (128, v.shape[-1])))
    rows1024 = np.concatenate([f["norm1_g"][0][None], f["cv_dw_b"][0][None], f["cv_ln_g"][0][None],
                               f["cv_ln_b"][0][None], f["norm2_g"][0][None], f["final_g"][None],
                               f["cv_dw_w"][0]], axis=0)
    rows3072 = np.concatenate([f["ssm_conv_b"][0][None], f["ssm_conv_w"][0]], axis=0)
    t_i = np.arange(128)[:, None]
    s_i = np.arange(128)[None, :]
    tri = np.stack([(t_i > s_i), (t_i <= s_i), np.where(s_i < t_i, NEGV, 0.0)], axis=1).astype(np.float32)
    xpad = np.zeros(((NCORES - 1) * T + HALO, D), np.float32)
    xpad[HALO:] = x[:(NCORES - 1) * T]
    common = {
        "xpad": xpad,
        "cT": np.ascontiguousarray(f["c"][0].reshape(8, 128).T),
        "w_ada": f["w_ada"][0], "b_ada": f["b_ada"][0][None], "w_in": f["w_in"][0],
        "pk1024": fm(rows1024, 8), "pk3072": fm(rows3072, 24),
        "w_cv_out": f["w_cv_out"][0],
        "ident": np.eye(128, dtype=np.float32),
        "tri": np.ascontiguousarray(tri),
        "rows32": np.concatenate([rep(f["dt_bias"][0]), rep(f["a_log"][0]), rep(f["d_skip"][0])], axis=1),
        "gn": np.ascontiguousarray(fm(f["ssm_norm_g"][0][None], 16)[:, :, 0]),
        "w_ssm_out": f["w_ssm_out"][0], "w_mix_out": f["w_mix_out"][0],
        "wr": np.ascontiguousarray(np.concatenate([f["w_grp"][0], f["w_er"][0]], axis=1)),
        "br": rep(np.concatenate([f["b_grp"][0], f["b_er"][0]])),
        "w1": f["w1"][0], "w3": f["w3"][0], "w2": f["w2"][0],
        "fg": rep(f["final_g"]),
    }
    maps = []
    for k in range(NCORES):
        xk = np.zeros((TH, D), np.float32)
        xk[HALO:] = x[k * T:(k + 1) * T]
        if k > 0:
            xk[:HALO] = x[k * T - HALO:k * T]
        m = dict(common)
        m["xh"] = xk
        m["hmask"] = np.full((128, 1), 0.0 if k == 0 else 1.0, np.float32)
        m["segmask"] = np.ascontiguousarray(np.broadcast_to((np.arange(8) < k).astype(np.float32)[None, :], (128, 8)))
        maps.append(m)
    return maps


def kernel(**inputs):
    maps = prep_inputs(inputs)
    nc = build_program("full")
    res = run_bass_kernel_spmd(nc, maps, core_ids=list(range(NCORES)))
    out = np.concatenate([np.asarray(res.results[k]["out"], np.float32) for k in range(NCORES)], axis=0)
    return out.reshape(1, SEQ, D)
```

```python
import functools
import contextlib
import numpy as np
import concourse.bass as bass
import concourse.mybir as mybir
from concourse.bass_utils import run_bass_kernel_spmd

F32 = mybir.dt.float32
BF16 = mybir.dt.bfloat16
I32 = mybir.dt.int32
U32 = mybir.dt.uint32
AF = mybir.ActivationFunctionType
ALU = mybir.AluOpType
AX = mybir.AxisListType

NCORES = 8
D = 1024
SEQ = 16384
T = SEQ // NCORES
NT = T // 128
HALO = 32
TH = T + HALO
DIN = 9248
C_GLU_A, C_GLU_B, C_Z, C_XBC, C_DT, C_GCV, C_GSSM = 0, 1024, 2048, 4096, 7168, 7200, 8224
EPS = 1e-6
ENGS = ("pe", "act", "dve", "pool", "sp")


class Buf:
    __slots__ = ("name", "w", "r")

    def __init__(self, name):
        self.name = name
        self.w = None
        self.r = []


class Sched:
    NDMA = 6

    def __init__(self, nc):
        self.nc = nc
        self.q = {e: [] for e in ENGS}
        self.sem = {e: nc.alloc_semaphore("prog_" + e) for e in ENGS}
        self.cnt = {e: 0 for e in ENGS}
        self.waited = {e: {} for e in ENGS}
        self.dq = ("sp", "pool", "act")
        self.dsem = {e: [nc.alloc_semaphore("dma_%s_%d" % (e, j)) for j in range(self.NDMA)]
                     for e in self.dq}
        self.dcnt = {e: [0] * self.NDMA for e in self.dq}
        self.didx = {e: 0 for e in self.dq}
        self.semkey = {}

    def _key(self, sem):
        k = id(sem)
        self.semkey[k] = sem
        return k

    def _waits(self, eng, reads, writes, skip_self):
        need = {}

        def add(ev):
            if ev is None:
                return
            sem, val = ev
            if skip_self and sem is self.sem[eng]:
                return
            k = self._key(sem)
            if need.get(k, 0) < val:
                need[k] = val
        for b in reads:
            add(b.w)
        for b in writes:
            add(b.w)
            for ev in b.r:
                add(ev)
        out = []
        wd = self.waited[eng]
        for k, val in need.items():
            if wd.get(k, 0) >= val:
                continue
            wd[k] = val
            out.append((self.semkey[k], val))
        return out

    def _emit_waits(self, eng, evs):
        for sem, val in evs:
            self.q[eng].append(functools.partial(lambda E, s, v: E.wait_ge(s, v), s=sem, v=val))

    def _record(self, ev, reads, writes):
        for b in reads:
            b.r.append(ev)
            if len(b.r) > 64:
                b.r = self._compact(b.r)
        for b in writes:
            b.w = ev
            b.r = []

    def _compact(self, evs):
        best = {}
        for sem, val in evs:
            k = self._key(sem)
            if best.get(k, 0) < val:
                best[k] = val
        return [(self.semkey[k], v) for k, v in best.items()]

    def op(self, eng, fn, reads=(), writes=()):
        evs = self._waits(eng, reads, writes, skip_self=(eng == "pe"))
        self._emit_waits(eng, evs)
        self.cnt[eng] += 1
        sem = self.sem[eng]
        self.q[eng].append(functools.partial(lambda E, f, s: f(E).then_inc(s, 1), f=fn, s=sem))
        ev = (sem, self.cnt[eng])
        self._record(ev, reads, writes)
        return ev

    def dma(self, eng, fn, reads=(), writes=()):
        j = self.didx[eng]
        self.didx[eng] = (j + 1) % self.NDMA
        sem = self.dsem[eng][j]
        prev = self.dcnt[eng][j]
        evs = self._waits(eng, reads, writes, skip_self=False)
        k = self._key(sem)
        if prev > 0 and self.waited[eng].get(k, 0) < 16 * prev:
            self.waited[eng][k] = 16 * prev
            evs.append((sem, 16 * prev))
        self._emit_waits(eng, evs)
        self.dcnt[eng][j] = prev + 1
        self.q[eng].append(functools.partial(lambda E, f, s: f(E).then_inc(s, 16), f=fn, s=sem))
        ev = (sem, 16 * (prev + 1))
        self._record(ev, reads, writes)
        return ev

    def barrier(self):
        evs = [(self.sem[e], self.cnt[e]) for e in ENGS if self.cnt[e] > 0]
        for e in self.dq:
            for j in range(self.NDMA):
                if self.dcnt[e][j] > 0:
                    evs.append((self.dsem[e][j], 16 * self.dcnt[e][j]))
        for eng in ENGS:
            wd = self.waited[eng]
            todo = []
            for sem, val in evs:
                if sem is self.sem[eng] and eng == "pe":
                    continue
                k = self._key(sem)
                if wd.get(k, 0) < val:
                    wd[k] = val
                    todo.append((sem, val))
            self._emit_waits(eng, todo)

    def finish(self, out_bufs):
        evs = self._waits("sp", out_bufs, (), skip_self=False)
        self._emit_waits("sp", evs)
        nc = self.nc
        q = self.q
        with nc.Block() as block:
            @block.tensor
            def _(E):
                for f in q["pe"]:
                    f(E)

            @block.scalar
            def _(E):
                for f in q["act"]:
                    f(E)

            @block.vector
            def _(E):
                for f in q["dve"]:
                    f(E)

            @block.gpsimd
            def _(E):
                for f in q["pool"]:
                    f(E)

            @block.sync
            def _(E):
                for f in q["sp"]:
                    f(E)


class Scope:
    _n = [0]

    def __init__(self, nc, S):
        self.nc = nc
        self.S = S
        self.es = contextlib.ExitStack()

    def __enter__(self):
        self.es.__enter__()
        return self

    def __exit__(self, *a):
        self.S.barrier()
        return self.es.__exit__(*a)

    def sb(self, name, shape, dt):
        Scope._n[0] += 1
        t = self.es.enter_context(self.nc.sbuf_tensor("%s_%d" % (name, Scope._n[0]), list(shape), dt))
        return t, Buf(name)


NEGV = -30000.0
BIG = 1.0e4
C_B = C_XBC + 2048
C_C = C_XBC + 2560


MOEQ = ("pool", "pool", "pool")


def build_program(stage="full", npre=NCORES - 1, nexp=64):
    nc = bass.Bass("TRN2", target_bir_lowering=False)
    S = Sched(nc)

    def din(name, shape, dt=F32):
        return nc.dram_tensor(name, list(shape), dt, kind="ExternalInput").ap()

    def dscr(name, shape, dt):
        return nc.dram_tensor(name, list(shape), dt, kind="Internal").ap()

    def wview(w, c0, n):
        return w[:, c0:c0 + n].rearrange("(k p) n -> p k n", p=128)

    def APX(ap, dims):
        return bass.AP(ap.tensor, ap.offset, [list(ap.ap[0])] + [list(d) for d in dims])

    def bc_in(ap, n):
        return APX(ap, [list(ap.ap[1]), [0, n]])

    def bc_mid(ap, r):
        return APX(ap, [[0, r], list(ap.ap[1])])

    def v3(ap, a, b):
        s = ap.ap[1][0]
        return APX(ap, [[b * s, a], [s, b]])

    def mmk(out, pairs, skip=False, start=True, stop=True):
        def f(E):
            last = None
            n = len(pairs)
            for i, (l, r) in enumerate(pairs):
                last = E.matmul(out, lhsT=l, rhs=r, start=(start and i == 0), stop=(stop and i == n - 1),
                                skip_group_check=skip)
            return last
        return f

    def mml(items):
        def f(E):
            last = None
            for (o, l, r, st, sp) in items:
                last = E.matmul(o, lhsT=l, rhs=r, start=st, stop=sp)
            return last
        return f

    def act(out, in_, func, **kw):
        return lambda E: E.activation(out=out, in_=in_, func=func, **kw)

    def tt(out, in0, in1, op):
        return lambda E: E.tensor_tensor(out=out, in0=in0, in1=in1, op=op)

    def ts(out, in0, s1, op0, s2=None, op1=None):
        if op1 is None:
            return lambda E: E.tensor_scalar(out=out, in0=in0, scalar1=s1, scalar2=None, op0=op0)
        return lambda E: E.tensor_scalar(out=out, in0=in0, scalar1=s1, scalar2=s2, op0=op0, op1=op1)

    def stt(out, in0, scalar, in1, op0, op1):
        return lambda E: E.scalar_tensor_tensor(out=out, in0=in0, scalar=scalar, in1=in1, op0=op0, op1=op1)

    def cp(out, in_):
        return lambda E: E.tensor_copy(out=out, in_=in_)

    def ms(out, v):
        return lambda E: E.memset(out, v)

    def dm(out, in_):
        return lambda E: E.dma_start(out=out, in_=in_)

    xh = din("xh", [TH, D])
    xpad = din("xpad", [(NCORES - 1) * T + HALO, D])
    hmask_d = din("hmask", [128, 1])
    segmask_d = din("segmask", [128, 8])
    cT_d = din("cT", [128, 8])
    w_ada = din("w_ada", [D, 6 * D])
    b_ada = din("b_ada", [1, 6 * D])
    w_in = din("w_in", [D, DIN])
    pk1024_d = din("pk1024", [128, 8, 37])
    pk3072_d = din("pk3072", [128, 24, 5])
    w_cv_out = din("w_cv_out", [D, D])
    ident_d = din("ident", [128, 128])
    tri_d = din("tri", [128, 3, 128])
    rows32_d = din("rows32", [128, 96])
    gn_d = din("gn", [128, 16])
    w_ssm_out = din("w_ssm_out", [2 * D, D])
    w_mix_out = din("w_mix_out", [D, D])
    wr_d = din("wr", [D, 72])
    br_d = din("br", [128, 72])
    w1_d = din("w1", [64, D, 512])
    w3_d = din("w3", [64, D, 512])
    w2_d = din("w2", [64, 512, D])
    fg_d = din("fg", [128, D])
    out_d = nc.dram_tensor("out", [T, D], F32, kind="ExternalOutput").ap()
    b_out = Buf("out")

    d_m1T = dscr("d_m1T", [128, 8, T], F32)
    b_dm1 = Buf("d_m1T")
    d_ynT = dscr("d_ynT", [128, 16, T], BF16)
    b_dyn = Buf("d_ynT")
    d_mT = dscr("d_mT", [128, 8, T], BF16)
    b_dmT = Buf("d_mT")
    d_h = dscr("d_h", [T, D], F32)
    b_dh = Buf("d_h")

    PB = []
    for i in range(8):
        PB.append((nc.alloc_psum_tensor("pb%d" % i, [128, 512], F32), Buf("pb%d" % i)))
    TS = [(PB[5 + s // 4][0][:, (s % 4) * 128:(s % 4 + 1) * 128], Buf("ts%d" % s)) for s in range(8)]

    def dbg_finish(items):
        outs = []
        for name, ap, shape, dt, bufs in items:
            o = nc.dram_tensor(name, list(shape), dt, kind="ExternalOutput").ap()
            bo = Buf(name)
            S.dma("sp", dm(o, ap), reads=bufs, writes=[bo])
            outs.append(bo)
        S.finish(outs)
        return nc

    L0 = Scope(nc, S)
    L0.__enter__()
    ident, b_ident = L0.sb("ident", [128, 128], F32)
    identb, b_identb = L0.sb("identb", [128, 128], BF16)
    onesb, b_onesb = L0.sb("onesb", [128, 128], BF16)
    onesf, b_onesf = L0.sb("onesf", [128, 128], F32)
    hmask, b_hmask = L0.sb("hmask", [128, 1], F32)
    pk1024, b_pk1024 = L0.sb("pk1024", [128, 8, 37], F32)
    modT, b_modT = L0.sb("modT", [128, 48], F32)
    g1bc, b_g1bc = L0.sb("g1bc", [128, D], F32)
    g2bc, b_g2bc = L0.sb("g2bc", [128, D], F32)
    s1col, b_s1col = L0.sb("s1col", [128, 8], F32)
    s2col, b_s2col = L0.sb("s2col", [128, 8], F32)
    gate, b_gate = L0.sb("gate", [128, NT, 64], F32)
    S.dma("sp", dm(ident[:], ident_d), writes=[b_ident])
    S.dma("sp", dm(hmask[:], hmask_d), writes=[b_hmask])
    S.dma("sp", dm(pk1024[:], pk1024_d), writes=[b_pk1024])
    S.op("dve", cp(identb[:], ident[:]), reads=[b_ident], writes=[b_identb])
    S.op("dve", ms(onesb[:], 1.0), writes=[b_onesb])
    S.op("dve", ms(onesf[:], 1.0), writes=[b_onesf])

    with Scope(nc, S) as sc:
        modrow, b_modrow = sc.sb("modrow", [1, 6 * D], F32)
        badar, b_badar = sc.sb("badar", [1, 6 * D], F32)
        cTs, b_cTs = sc.sb("cTs", [128, 8], F32)
        scb, b_scb = sc.sb("scb", [128, 8], BF16)
        wada = [sc.sb("wada%d" % i, [128, 8, 512], BF16) for i in range(2)]
        S.dma("sp", dm(cTs[:], cT_d), writes=[b_cTs])
        S.dma("sp", dm(badar[:], b_ada), writes=[b_badar])
        S.op("act", act(scb[:], cTs[:], AF.Silu), reads=[b_cTs], writes=[b_scb])
        for cb in range(12):
            wt, bw = wada[cb % 2]
            S.dma("pool", dm(wt[:], wview(w_ada, cb * 512, 512)), writes=[bw])
            pt_, bp = PB[cb % 2]
            S.op("pe", mmk(pt_[0:1, :], [(scb[:, k:k + 1], wt[:, k, :]) for k in range(8)]), reads=[b_scb, bw], writes=[bp])
            S.op("dve", tt(modrow[0:1, cb * 512:(cb + 1) * 512], pt_[0:1, :], badar[0:1, cb * 512:(cb + 1) * 512], ALU.add),
                 reads=[bp, b_badar], writes=[b_modrow])
        pcol, bpcol = PB[2]
        S.op("pe", mml([(pcol[:, j:j + 1], modrow[0:1, j * 128:(j + 1) * 128], onesf[0:1, 0:1], True, True)
                        for j in range(48)]), reads=[b_modrow, b_onesf], writes=[bpcol])
        S.op("dve", cp(modT[:], pcol[:, 0:48]), reads=[bpcol], writes=[b_modT])
        for (dst, bd, base) in ((g1bc, b_g1bc, 2 * D), (g2bc, b_g2bc, 5 * D)):
            for hb in range(2):
                pt_, bp = PB[3 + hb]
                S.op("pe", mmk(pt_[:], [(onesf[0:1, :], modrow[0:1, base + hb * 512: base + (hb + 1) * 512])]),
                     reads=[b_modrow, b_onesf], writes=[bp])
                S.op("act", act(dst[:, hb * 512:(hb + 1) * 512], pt_[:], AF.Copy), reads=[bp], writes=[bd])
        S.op("dve", stt(s1col[:], modT[:, 8:16], 1.0, pk1024[:, :, 0], ALU.add, ALU.mult),
             reads=[b_modT, b_pk1024], writes=[b_s1col])
        S.op("dve", stt(s2col[:], modT[:, 32:40], 1.0, pk1024[:, :, 4], ALU.add, ALU.mult),
             reads=[b_modT, b_pk1024], writes=[b_s2col])

    def stage1(sc, src, n1T, b_n1T):
        xt_bufs = [sc.sb("xt%d" % i, [128, D], F32) for i in range(2)]
        xn_bufs = [sc.sb("xn%d" % i, [128, D], BF16) for i in range(2)]
        sq_t, b_sq = sc.sb("sq", [128, D], BF16)
        st_bufs = [sc.sb("st%d" % i, [128, 2], F32) for i in range(2)]
        for i in range(NT + 1):
            rows = HALO if i == 0 else 128
            r0 = 0 if i == 0 else HALO + (i - 1) * 128
            xt, bxt = xt_bufs[i % 2]
            xn, bxn = xn_bufs[i % 2]
            st, bst = st_bufs[i % 2]
            S.dma("sp", dm(xt[0:rows, :], src[r0:r0 + rows, :]), writes=[bxt])
            S.op("act", act(sq_t[0:rows, :], xt[0:rows, :], AF.Square, scale=1.0 / 32, accum_out=st[0:rows, 0:1]),
                 reads=[bxt], writes=[b_sq, bst])
            S.op("act", act(st[0:rows, 1:2], st[0:rows, 0:1], AF.Sqrt, bias=EPS), reads=[bst], writes=[bst])
            S.op("dve", lambda E, st=st, rows=rows: E.reciprocal(out=st[0:rows, 1:2], in_=st[0:rows, 1:2]),
                 reads=[bst], writes=[bst])
            S.op("dve", ts(xn[0:rows, :], xt[0:rows, :], st[0:rows, 1:2], ALU.mult), reads=[bxt, bst], writes=[bxn])
            for k in range(8):
                tsa, ptb = TS[k]
                S.op("pe", mmk(tsa[:, 0:rows], [(xn[0:rows, k * 128:(k + 1) * 128], identb[0:rows, 0:rows])]),
                     reads=[bxn, b_identb], writes=[ptb])
                S.op("dve", ts(n1T[:, k, r0:r0 + rows], tsa[:, 0:rows], s1col[:, k:k + 1], ALU.mult,
                               modT[:, k:k + 1], ALU.add),
                     reads=[ptb, b_s1col, b_modT], writes=[b_n1T[i]])

    def n1_bufs_for(b_n1T, c0, n):
        out = []
        for i in range(NT + 1):
            lo = 0 if i == 0 else HALO + (i - 1) * 128
            hi = HALO if i == 0 else lo + 128
            if lo < c0 + n and hi > c0:
                out.append(b_n1T[i])
        return out

    TBLK = [(0, HALO)] + [(HALO + 512 * j, 512) for j in range(4)]

    LA = Scope(nc, S)
    LA.__enter__()
    n1T, _ = LA.sb("n1T", [128, 8, TH], BF16)
    b_n1T = [Buf("n1T_%d" % i) for i in range(NT + 1)]

    LS = Scope(nc, S)
    LS.__enter__()
    pk3072, b_pk3072 = LS.sb("pk3072", [128, 24, 5], F32)
    tri, b_tri = LS.sb("tri", [128, 3, 128], F32)
    Ub, b_Ub = LS.sb("Ub", [128, 128], BF16)
    Vb, b_Vb = LS.sb("Vb", [128, 128], BF16)
    NEGB, b_NEGB = LS.sb("NEGB", [128, 512], BF16)
    rows32, b_rows = LS.sb("rows32", [128, 96], F32)
    arow, b_arow = LS.sb("arow", [128, 32], F32)
    gn, b_gn = LS.sb("gn", [128, 16], F32)
    segmask, b_segmask = LS.sb("segmask", [128, 8], F32)
    wdt, b_wdt = LS.sb("wdt", [128, 8, 32], BF16)
    stf, b_stf = LS.sb("stf", [128, 2048], F32)
    stb, b_stb = LS.sb("stb", [128, 2048], BF16)
    b_stfg = [Buf("stf%d" % g) for g in range(4)]
    b_stbg = [Buf("stb%d" % g) for g in range(4)]
    S.dma("sp", dm(pk3072[:], pk3072_d), writes=[b_pk3072])
    S.dma("sp", dm(tri[:], tri_d), writes=[b_tri])
    S.dma("sp", dm(rows32[:], rows32_d), writes=[b_rows])
    S.dma("sp", dm(gn[:], gn_d), writes=[b_gn])
    S.dma("sp", dm(segmask[:], segmask_d), writes=[b_segmask])
    S.dma("pool", dm(wdt[:], wview(w_in, C_DT, 32)), writes=[b_wdt])
    S.op("dve", cp(Ub[:], tri[:, 0, :]), reads=[b_tri], writes=[b_Ub])
    S.op("dve", cp(Vb[:], tri[:, 1, :]), reads=[b_tri], writes=[b_Vb])
    S.op("dve", cp(v3(NEGB[:], 4, 128), bc_mid(tri[:, 2, :], 4)), reads=[b_tri], writes=[b_NEGB])
    S.op("act", act(arow[:], rows32[:, 32:64], AF.Exp), reads=[b_rows], writes=[b_arow])
    S.op("dve", ts(arow[:], arow[:], -1.0, ALU.mult), reads=[b_arow], writes=[b_arow])
    S.op("dve", ms(stf[:], 0.0), writes=b_stfg)
    Uf, Vf = tri[:, 0, :], tri[:, 1, :]
    dtb, dsk = rows32[:, 0:32], rows32[:, 64:96]

    def dt_pipe(sc, n1T, b_n1T, pre, maskcol=None):
        R = {}
        R["dt"], R["b_dt"] = sc.sb("dt", [128, NT, 32], F32)
        R["ad"], R["b_ad"] = sc.sb("ad", [128, NT, 32], F32)
        R["EX"], R["b_EX"] = sc.sb("EX", [128, NT, 96], F32)
        R["wgt"], R["b_wgt"] = sc.sb("wgt", [128, NT, 32], F32)
        R["Dj"], R["b_Dj"] = sc.sb("Dj", [128, 32], F32)
        t0, bt0 = sc.sb("dt_t0", [128, NT, 32], F32)
        sfx, bsfx = sc.sb("dt_sfx", [128, NT + 1, 32], F32)
        tots, btots = sc.sb("dt_tots", [128, NT, 32], F32)
        dt, ad, EX, wgt = R["dt"], R["ad"], R["EX"], R["wgt"]
        b_dt, b_ad, b_EX, b_wgt = R["b_dt"], R["b_ad"], R["b_EX"], R["b_wgt"]
        pd, bpd = PB[3]
        items = []
        for i in range(NT):
            tsl = slice(HALO + i * 128, HALO + (i + 1) * 128)
            for k in range(8):
                items.append((pd[:, i * 32:(i + 1) * 32], n1T[:, k, tsl], wdt[:, k, :], k == 0, k == 7))
        S.op("pe", mml(items), reads=[b_wdt] + b_n1T[1:], writes=[bpd])
        S.op("dve", tt(t0[:], v3(pd[:], NT, 32), bc_mid(dtb, NT), ALU.add), reads=[bpd, b_rows], writes=[bt0])
        S.op("act", act(t0[:], t0[:], AF.Exp), reads=[bt0], writes=[bt0])
        S.op("act", act(dt[:], t0[:], AF.Ln, bias=1.0), reads=[bt0], writes=[b_dt])
        if pre:
            S.op("dve", ts(dt[:], dt[:], maskcol, ALU.mult), reads=[b_dt, b_segmask], writes=[b_dt])
        S.op("dve", tt(ad[:], dt[:], bc_mid(arow[:], NT), ALU.mult), reads=[b_dt, b_arow], writes=[b_ad])
        adf = APX(ad[:], [[1, NT * 32]])
        (p4, bp4), (p5, bp5), (p6, bp6) = PB[4], PB[5], PB[6]
        S.op("pe", mml([(p4[:], Uf, adf, True, True), (p5[:], Vf, adf, True, True), (p6[:], onesf[:], adf, True, True)]),
             reads=[b_ad, b_tri, b_onesf], writes=[bp4, bp5, bp6])
        if not pre:
            S.op("act", act(EX[:, :, 0:32], v3(p4[:], NT, 32), AF.Exp), reads=[bp4], writes=[b_EX])
            S.op("act", act(EX[:, :, 32:64], v3(p5[:], NT, 32), AF.Exp), reads=[bp5], writes=[b_EX])
            S.op("act", act(EX[:, :, 64:96], v3(p6[:], NT, 32), AF.Exp), reads=[bp6], writes=[b_EX])
        else:
            S.op("dve", cp(tots[:], v3(p6[:], NT, 32)), reads=[bp6], writes=[btots])
            S.op("dve", ms(sfx[:, NT - 1:NT + 1, :], 0.0), writes=[bsfx])
            for i in range(NT - 2, -2, -1):
                dst = sfx[:, i, :] if i >= 0 else sfx[:, NT, :]
                S.op("dve", tt(dst, sfx[:, i + 1, :], tots[:, i + 1, :], ALU.add), reads=[bsfx, btots], writes=[bsfx])
            S.op("dve", tt(t0[:], v3(p4[:], NT, 32), sfx[:, 0:NT, :], ALU.add), reads=[bp4, bsfx, bt0], writes=[bt0])
            S.op("act", act(t0[:], t0[:], AF.Exp), reads=[bt0], writes=[bt0])
            S.op("dve", tt(wgt[:], t0[:], dt[:], ALU.mult), reads=[bt0, b_dt], writes=[b_wgt])
            S.op("act", act(R["Dj"][:], sfx[:, NT, :], AF.Exp), reads=[bsfx], writes=[R["b_Dj"]])
        return R

    def dt_pipe_old(sc, n1T, b_n1T, pre, maskcol=None):
        R = {}
        R["dt"], R["b_dt"] = sc.sb("dt", [128, NT, 32], F32)
        R["ad"], R["b_ad"] = sc.sb("ad", [128, NT, 32], F32)
        R["EX"], R["b_EX"] = sc.sb("EX", [128, NT, 96], F32)
        R["wgt"], R["b_wgt"] = sc.sb("wgt", [128, NT, 32], F32)
        R["run"], R["b_run"] = sc.sb("run", [128, 32], F32)
        R["Dj"], R["b_Dj"] = sc.sb("Dj", [128, 32], F32)
        t0s = [sc.sb("dt_t0_%d" % i, [128, 32], F32) for i in range(2)]
        t1s = [sc.sb("dt_t1_%d" % i, [128, 32], F32) for i in range(2)]
        dt, ad, EX, wgt, run = R["dt"], R["ad"], R["EX"], R["wgt"], R["run"]
        b_dt, b_ad, b_EX, b_wgt, b_run = R["b_dt"], R["b_ad"], R["b_EX"], R["b_wgt"], R["b_run"]
        pd, bpd = PB[6]
        pc, bpc = PB[7]
        if pre:
            S.op("dve", ms(run[:], 0.0), writes=[b_run])
        order = range(NT - 1, -1, -1) if pre else range(NT)
        for n_, i in enumerate(order):
            tsl = slice(HALO + i * 128, HALO + (i + 1) * 128)
            t0, bt0 = t0s[n_ % 2]
            t1, bt1 = t1s[n_ % 2]
            S.op("pe", mmk(pd[:, 0:32], [(n1T[:, k, tsl], wdt[:, k, :]) for k in range(8)]),
                 reads=[b_n1T[i + 1], b_wdt], writes=[bpd])
            S.op("dve", tt(t0[:], pd[:, 0:32], dtb, ALU.add), reads=[bpd, b_rows], writes=[bt0])
            S.op("act", act(t0[:], t0[:], AF.Exp), reads=[bt0], writes=[bt0])
            S.op("act", act(dt[:, i, :], t0[:], AF.Ln, bias=1.0), reads=[bt0], writes=[b_dt])
            if pre:
                S.op("dve", ts(dt[:, i, :], dt[:, i, :], maskcol, ALU.mult), reads=[b_dt, b_segmask], writes=[b_dt])
            S.op("dve", tt(ad[:, i, :], dt[:, i, :], arow[:], ALU.mult), reads=[b_dt, b_arow], writes=[b_ad])
            S.op("pe", mml([(pc[:, 0:32], Uf, ad[:, i, :], True, True),
                            (pc[:, 32:64], Vf, ad[:, i, :], True, True),
                            (pc[:, 64:96], onesf[:], ad[:, i, :], True, True)]),
                 reads=[b_ad, b_tri, b_onesf], writes=[bpc])
            if not pre:
                S.op("act", act(EX[:, i, :], pc[:, 0:96], AF.Exp), reads=[bpc], writes=[b_EX])
            else:
                S.op("dve", tt(t1[:], pc[:, 0:32], run[:], ALU.add), reads=[bpc, b_run], writes=[bt1])
                S.op("act", act(t1[:], t1[:], AF.Exp), reads=[bt1], writes=[bt1])
                S.op("dve", tt(wgt[:, i, :], t1[:], dt[:, i, :], ALU.mult), reads=[bt1, b_dt], writes=[b_wgt])
                S.op("dve", tt(run[:], run[:], pc[:, 64:96], ALU.add), reads=[bpc, b_run], writes=[b_run])
        if pre:
            S.op("act", act(R["Dj"][:], run[:], AF.Exp), reads=[b_run], writes=[R["b_Dj"]])
        return R

    def prep_group(sc, n1T, b_n1T, g, nch, halo_mode):
        chunk_ids = [g * 4 + cc for cc in range(4)] + [16 + g] + ([20 + g] if nch == 6 else [])
        wblk, b_wblk = sc.sb("wblk", [128, 8, nch * 128], BF16)
        dg, _ = sc.sb("dg4", [128, nch * 4, 128], BF16)
        b_dg = [Buf("dg4_%d" % cc) for cc in range(nch)]
        xpost, _ = sc.sb("xpost", [128, nch, T], BF16)
        b_xpost = [[Buf("xpost_%d_%d" % (cc, j)) for j in range(4)] for cc in range(nch)]
        xpre_bufs = [sc.sb("xpre%d" % i, [128, TH], BF16) for i in range(2)]
        S.dma("pool", dm(wblk[:, :, 0:512], wview(w_in, C_XBC + g * 512, 512)), writes=[b_wblk])
        S.dma("pool", dm(wblk[:, :, 512:640], wview(w_in, C_B + g * 128, 128)), writes=[b_wblk])
        if nch == 6:
            S.dma("pool", dm(wblk[:, :, 640:768], wview(w_in, C_C + g * 128, 128)), writes=[b_wblk])
        for cc in range(nch):
            for k in range(4):
                S.op("dve", ts(dg[:, cc * 4 + k, :], identb[:], pk3072[:, chunk_ids[cc], 1 + k:2 + k], ALU.mult),
                     reads=[b_identb, b_pk3072], writes=[b_dg[cc]])
        for cc in range(nch):
            xp, bxp = xpre_bufs[cc % 2]
            for jb, (c0, n) in enumerate(TBLK):
                pt_, bp = PB[jb % 2]
                if jb == 0 and halo_mode == "zero":
                    S.op("dve", ms(xp[:, 0:HALO], 0.0), writes=[bxp])
                    continue
                S.op("pe", mmk(pt_[:, 0:n], [(wblk[:, k, cc * 128:(cc + 1) * 128], n1T[:, k, c0:c0 + n]) for k in range(8)]),
                     reads=[b_wblk] + n1_bufs_for(b_n1T, c0, n), writes=[bp])
                if jb == 0 and halo_mode == "mask":
                    S.op("dve", ts(xp[:, 0:n], pt_[:, 0:n], hmask[:, 0:1], ALU.mult), reads=[bp, b_hmask], writes=[bxp])
                else:
                    S.op("dve", cp(xp[:, c0:c0 + n], pt_[:, 0:n]), reads=[bp], writes=[bxp])
            for j in range(4):
                c0 = HALO + 512 * j
                pc_, bpc = PB[2 + j % 2]
                S.op("pe", mmk(pc_[:], [(dg[:, cc * 4 + k, :], xp[:, c0 - 3 + k:c0 - 3 + k + 512]) for k in range(4)]),
                     reads=[b_dg[cc], bxp], writes=[bpc])
                S.op("act", act(xpost[:, cc, 512 * j:512 * (j + 1)], pc_[:], AF.Silu, bias=pk3072[:, chunk_ids[cc], 0:1]),
                     reads=[bpc, b_pk3072], writes=[b_xpost[cc][j]])
        return xpost, b_xpost

    b_p7h = [Buf("p7h0"), Buf("p7h1")]
    for jseg in range(npre):
        with Scope(nc, S) as sp:
            with Scope(nc, S) as s1:
                stage1(s1, xpad[jseg * T:jseg * T + TH, :], n1T, b_n1T)
            with Scope(nc, S) as s2:
                R = dt_pipe(s2, n1T, b_n1T, True, segmask[:, jseg:jseg + 1])
                xd_bufs = [s2.sb("xdp%d" % i, [128, 512], BF16) for i in range(2)]
                bt_bufs = [s2.sb("btp%d" % i, [128, 128], BF16) for i in range(2)]
                for g in range(4):
                    with Scope(nc, S) as s3:
                        xpost, b_xpost = prep_group(s3, n1T, b_n1T, g, 5, "zero" if jseg == 0 else "plain")
                        pst, bpst = PB[4]
                        for i in range(NT):
                            tsl = slice(i * 128, (i + 1) * 128)
                            px, bpx = PB[5]
                            pbt, bpbt = PB[6]
                            xd, bxd = xd_bufs[i % 2]
                            bt, bbt = bt_bufs[i % 2]
                            S.op("pe", mml([(px[:, cc * 128:(cc + 1) * 128], xpost[:, cc, tsl], identb[:], True, True)
                                            for cc in range(4)]),
                                 reads=[b_identb] + [b_xpost[cc][i // 4] for cc in range(4)], writes=[bpx])
                            S.op("pe", mmk(pbt[:, 0:128], [(xpost[:, 4, tsl], identb[:])]),
                                 reads=[b_identb, b_xpost[4][i // 4]], writes=[bpbt])
                            S.op("dve", tt(v3(xd[:], 8, 64), v3(px[:], 8, 64), bc_in(R["wgt"][:, i, g * 8:(g + 1) * 8], 64), ALU.mult),
                                 reads=[bpx, R["b_wgt"]], writes=[bxd])
                            S.op("act", act(bt[:], pbt[:, 0:128], AF.Copy), reads=[bpbt], writes=[bbt])
                            S.op("pe", mmk(pst[:], [(bt[:], xd[:])], skip=True, start=(i == 0), stop=(i == NT - 1)),
                                 reads=[bbt, bxd], writes=[bpst])
                        sl = slice(g * 512, (g + 1) * 512)
                        S.op("pool", tt(v3(stf[:, sl], 8, 64), v3(stf[:, sl], 8, 64), bc_in(R["Dj"][:, g * 8:(g + 1) * 8], 64), ALU.mult),
                             reads=[b_stfg[g], R["b_Dj"]], writes=[b_stfg[g]])
                        S.op("dve", tt(stf[:, sl], stf[:, sl], pst[:], ALU.add), reads=[b_stfg[g], bpst], writes=[b_stfg[g]])

    if stage == "PRE":
        return dbg_finish([("o_stf", stf[:], [128, 2048], F32, b_stfg)])

    with Scope(nc, S) as sc:
        stage1(sc, xh, n1T, b_n1T)

    for g in range(4):
        S.op("act", act(stb[:, g * 512:(g + 1) * 512], stf[:, g * 512:(g + 1) * 512], AF.Copy),
             reads=[b_stfg[g]], writes=[b_stbg[g]])
    with Scope(nc, S) as s2:
        R = dt_pipe(s2, n1T, b_n1T, False)
        dt, ad, EX = R["dt"], R["ad"], R["EX"]
        wz_t, b_wz = s2.sb("wz", [128, 8, 512], BF16)

        def tb(name, shape, dtp, n=2):
            return [s2.sb("%s%d" % (name, i), shape, dtp) for i in range(n)]
        xdt_b, xsD_b, xd_b = tb("xdt", [128, 512], BF16), tb("xsD", [128, 512], BF16), tb("xd", [128, 512], BF16)
        bt_b, cb_b = tb("btk", [128, 128], BF16), tb("cbs", [128, 128], BF16)
        adV_b, LT_b, MT_b = tb("adV", [128, 1024], BF16), tb("LT", [128, 1024], BF16), tb("MT", [128, 1024], BF16)
        t1_b, ys_b, zs_b, yz_b = tb("t1", [128, 512], F32), tb("ysb", [128, 512], F32), tb("zs", [128, 512], F32), tb("yz", [128, 512], F32)
        sq_b, yn_b = tb("sqj", [128, 512], BF16), tb("yn", [128, 512], BF16)
        ss_b = tb("ss", [128, 2], F32)
        ynT_b = tb("ynTt", [128, 4, 128], BF16)
        it = 0
        pending = []
        for g in range(4):
            g8 = slice(g * 8, (g + 1) * 8)
            gsl = slice(g * 512, (g + 1) * 512)
            with Scope(nc, S) as s3:
                xpost, b_xpost = prep_group(s3, n1T, b_n1T, g, 6, "mask")
                S.dma("pool", dm(wz_t[:], wview(w_in, C_Z + g * 512, 512)), writes=[b_wz])
                for i in range(NT):
                    q = it % 2
                    it += 1
                    tsl = slice(i * 128, (i + 1) * 128)
                    hsl = slice(HALO + i * 128, HALO + (i + 1) * 128)
                    rx = [b_xpost[cc][i // 4] for cc in range(4)]
                    rB, rC = b_xpost[4][i // 4], b_xpost[5][i // 4]
                    (xdt, bxdt), (xsD, bxsD), (xd, bxd) = xdt_b[q], xsD_b[q], xd_b[q]
                    (bt, bbt), (cbs, bcbs) = bt_b[q], cb_b[q]
                    (adV, badV), (LT, bLT), (MT, bMT) = adV_b[q], LT_b[q], MT_b[q]
                    (t1, bt1), (ysb, bys), (zs, bzs), (yz, byz) = t1_b[q], ys_b[q], zs_b[q], yz_b[q]
                    (sqj, bsq), (yn, byn), (ss, bss), (ynTt, bynT) = sq_b[q], yn_b[q], ss_b[q], ynT_b[q]
                    px, bpx = PB[2]
                    p3, bp3 = PB[3]
                    S.op("pe", mml([(px[:, cc * 128:(cc + 1) * 128], xpost[:, cc, tsl], identb[:], True, True) for cc in range(4)]),
                         reads=[b_identb] + rx, writes=[bpx])
                    S.op("pe", mml([(p3[:, 0:128], xpost[:, 4, tsl], identb[:], True, True),
                                    (p3[:, 128:256], xpost[:, 4, tsl], xpost[:, 5, tsl], True, True)]),
                         reads=[b_identb, rB, rC], writes=[bp3])
                    S.op("dve", tt(v3(xdt[:], 8, 64), v3(px[:], 8, 64), bc_in(dt[:, i, g8], 64), ALU.mult),
                         reads=[bpx, R["b_dt"]], writes=[bxdt])
                    S.op("dve", tt(v3(xsD[:], 8, 64), v3(px[:], 8, 64), bc_in(rows32[:, 64 + g * 8:72 + g * 8], 64), ALU.mult),
                         reads=[bpx, b_rows], writes=[bxsD])
                    S.op("dve", tt(v3(xd[:], 8, 64), v3(xdt[:], 8, 64), bc_in(EX[:, i, g * 8:(g + 1) * 8], 64), ALU.mult),
                         reads=[bxdt, R["b_EX"]], writes=[bxd])
                    S.op("act", act(bt[:], p3[:, 0:128], AF.Copy), reads=[bp3], writes=[bbt])
                    S.op("act", act(cbs[:], p3[:, 128:256], AF.Copy), reads=[bp3], writes=[bcbs])
                    S.op("dve", tt(v3(adV[:], 8, 128), bc_mid(Vb[:], 8), bc_in(ad[:, i, g8], 128), ALU.mult),
                         reads=[b_Vb, R["b_ad"]], writes=[badV])
                    for half in range(2):
                        ph, bph = PB[half]
                        S.op("pe", mml([(ph[:], Ub[:], adV[:, half * 512:(half + 1) * 512], True, False),
                                        (ph[:], identb[:], NEGB[:], False, True)]),
                             reads=[b_Ub, badV, b_identb, b_NEGB], writes=[bph])
                        S.op("act", act(LT[:, half * 512:(half + 1) * 512], ph[:], AF.Exp), reads=[bph], writes=[bLT])
                    S.op("dve", tt(v3(MT[:], 8, 128), v3(LT[:], 8, 128), bc_mid(cbs[:], 8), ALU.mult),
                         reads=[bLT, bcbs], writes=[bMT])
                    py, bpy = PB[4]
                    po, bpo = PB[5]
                    pst, bpst = PB[6]
                    items = []
                    for r in range(8):
                        rs = slice(r * 64, (r + 1) * 64)
                        items.append((py[:, rs], identb[:], xsD[:, rs], True, False))
                        items.append((py[:, rs], MT[:, r * 128:(r + 1) * 128], xdt[:, rs], False, True))
                    S.op("pe", mml(items), reads=[b_identb, bxsD, bMT, bxdt], writes=[bpy])
                    S.op("pe", mmk(po[:], [(xpost[:, 5, tsl], stb[:, gsl])]), reads=[rC, b_stbg[g]], writes=[bpo])
                    S.op("pe", mmk(pst[:], [(bt[:], xd[:])]), reads=[bbt, bxd], writes=[bpst])
                    S.op("dve", tt(v3(t1[:], 8, 64), v3(po[:], 8, 64), bc_in(EX[:, i, 32 + g * 8:40 + g * 8], 64), ALU.mult),
                         reads=[bpo, R["b_EX"]], writes=[bt1])
                    S.op("dve", tt(ysb[:], py[:], t1[:], ALU.add), reads=[bpy, bt1], writes=[bys])
                    S.op("dve", tt(v3(stf[:, gsl], 8, 64), v3(stf[:, gsl], 8, 64), bc_in(EX[:, i, 64 + g * 8:72 + g * 8], 64), ALU.mult),
                         reads=[b_stfg[g], R["b_EX"]], writes=[b_stfg[g]])
                    S.op("dve", tt(stf[:, gsl], stf[:, gsl], pst[:], ALU.add), reads=[b_stfg[g], bpst], writes=[b_stfg[g]])
                    S.op("dve", cp(stb[:, gsl], stf[:, gsl]), reads=[b_stfg[g]], writes=[b_stbg[g]])
                    pz, bpz = PB[7]
                    S.op("pe", mmk(pz[:], [(n1T[:, k, hsl], wz_t[:, k, :]) for k in range(8)]),
                         reads=[b_n1T[i + 1], b_wz], writes=[bpz])
                    S.op("act", act(zs[:], pz[:], AF.Silu), reads=[bpz], writes=[bzs])
                    S.op("dve", tt(yz[:], ysb[:], zs[:], ALU.mult), reads=[bys, bzs], writes=[byz])
                    S.op("act", act(sqj[:], yz[:], AF.Square, scale=float(512 ** -0.5), accum_out=ss[:, 0:1]),
                         reads=[byz], writes=[bsq, bss])
                    S.op("act", act(ss[:, 1:2], ss[:, 0:1], AF.Sqrt, bias=EPS), reads=[bss], writes=[bss])
                    S.op("dve", lambda E, ss=ss: E.reciprocal(out=ss[:, 1:2], in_=ss[:, 1:2]), reads=[bss], writes=[bss])
                    S.op("dve", ts(yn[:], yz[:], ss[:, 1:2], ALU.mult), reads=[byz, bss], writes=[byn])

                    def tail(g=g, tsl=tsl, yn=yn, byn=byn, ynTt=ynTt, bynT=bynT):
                        pt_, bpt_ = PB[2]
                        S.op("pe", mml([(pt_[:, cc * 128:(cc + 1) * 128], yn[:, cc * 128:(cc + 1) * 128], identb[:], True, True)
                                        for cc in range(4)]), reads=[byn, b_identb], writes=[bpt_])
                        S.op("dve", tt(ynTt[:], v3(pt_[:], 4, 128), bc_in(gn[:, g * 4:(g + 1) * 4], 128), ALU.mult),
                             reads=[bpt_, b_gn], writes=[bynT])
                        S.dma("sp", dm(d_ynT[:, g * 4:(g + 1) * 4, tsl], ynTt[:]), reads=[bynT], writes=[b_dyn])
                    tail()
                    if i == NT - 1:
                        while len(pending) > 0:
                            pending.pop(0)()
                    if stage == "B1" and g == 0 and i == 1:
                        return dbg_finish([("o_ys", ysb[:], [128, 512], F32, [bys]),
                                           ("o_yn", yn[:], [128, 512], BF16, [byn])])
    LS.__exit__(None, None, None)
    if stage == "B":
        with Scope(nc, S) as sc:
            yt, byt = sc.sb("ynall", [128, 16, T], BF16)
            S.dma("sp", dm(yt[:], d_ynT), reads=[b_dyn], writes=[byt])
            return dbg_finish([("o_ynT", yt[:], [128, 16, T], BF16, [byt])])

    LB = Scope(nc, S)
    LB.__enter__()
    cuT, _ = LB.sb("cuT", [128, 8, T], BF16)
    b_cuT = [[Buf("cuT_%d_%d" % (c, j)) for j in range(4)] for c in range(8)]
    with Scope(nc, S) as scu:
        uT, _ = scu.sb("uT", [128, 8, TH], BF16)
        b_uT = [[Buf("uT_%d_%d" % (c, j)) for j in range(5)] for c in range(8)]
        with Scope(nc, S) as sc:
            wg_bufs = [sc.sb("wg%d" % i, [128, 8, 512], BF16) for i in range(4)]
            sg_bufs = [sc.sb("sg%d" % i, [128, 512], F32) for i in range(2)]
            ecnt = 0
            for blk in range(2):
                wa, bwa = wg_bufs[(2 * blk) % 4]
                wb, bwb = wg_bufs[(2 * blk + 1) % 4]
                S.dma("pool", dm(wa[:], wview(w_in, C_GLU_A + blk * 512, 512)), writes=[bwa])
                S.dma("pool", dm(wb[:], wview(w_in, C_GLU_B + blk * 512, 512)), writes=[bwb])
                for cc in range(4):
                    c = blk * 4 + cc
                    for j, (c0, n) in enumerate(TBLK):
                        pa, bpa = PB[(2 * ecnt) % 4]
                        pb_, bpb = PB[(2 * ecnt + 1) % 4]
                        sg, bsg = sg_bufs[ecnt % 2]
                        ecnt += 1
                        rb = n1_bufs_for(b_n1T, c0, n)
                        S.op("pe", mml([(pa[:, 0:n], wa[:, k, cc * 128:(cc + 1) * 128], n1T[:, k, c0:c0 + n], k == 0, k == 7)
                                        for k in range(8)] +
                                       [(pb_[:, 0:n], wb[:, k, cc * 128:(cc + 1) * 128], n1T[:, k, c0:c0 + n], k == 0, k == 7)
                                        for k in range(8)]), reads=[bwa, bwb] + rb, writes=[bpa, bpb])
                        S.op("act", act(sg[:, 0:n], pb_[:, 0:n], AF.Sigmoid), reads=[bpb], writes=[bsg])
                        if j == 0:
                            S.op("dve", stt(uT[:, c, c0:c0 + n], pa[:, 0:n], hmask[:, 0:1], sg[:, 0:n], ALU.mult, ALU.mult),
                                 reads=[bpa, bsg, b_hmask], writes=[b_uT[c][j]])
                        else:
                            S.op("dve", tt(uT[:, c, c0:c0 + n], pa[:, 0:n], sg[:, 0:n], ALU.mult),
                                 reads=[bpa, bsg], writes=[b_uT[c][j]])
        with Scope(nc, S) as sc:
            dg_bufs = [sc.sb("dg%d" % i, [128, 31, 128], BF16) for i in range(2)]
            csq_bufs = [sc.sb("csq%d" % i, [128, 512], BF16) for i in range(2)]
            ssum, b_ssum = sc.sb("ssum", [128, T], F32)
            ssq, b_ssq = sc.sb("ssq", [128, T], F32)
            tmp_bufs = [sc.sb("lnt%d" % i, [128, 512], F32) for i in range(2)]
            for c in range(8):
                dg, b_dg = dg_bufs[c % 2]
                for k in range(31):
                    eng = "dve"
                    S.op(eng, ts(dg[:, k, :], identb[:], pk1024[:, c, 6 + k:7 + k], ALU.mult),
                         reads=[b_identb, b_pk1024], writes=[b_dg])
                for j in range(4):
                    c0 = HALO + 512 * j
                    pc, bpc = PB[j % 2]
                    ps1, bps1 = PB[2] if j % 2 == 0 else PB[7]
                    ps2, bps2 = PB[3] if j % 2 == 0 else PB[4]
                    csq, bcsq = csq_bufs[j % 2]
                    S.op("pe", mmk(pc[:], [(dg[:, k, :], uT[:, c, c0 - 30 + k: c0 - 30 + k + 512]) for k in range(31)]),
                         reads=[b_dg, b_uT[c][j + 1], b_uT[c][j]], writes=[bpc])
                    S.op("act", act(cuT[:, c, 512 * j:512 * (j + 1)], pc[:], AF.Identity, bias=pk1024[:, c, 1:2]),
                         reads=[bpc, b_pk1024], writes=[b_cuT[c][j]])
                    S.op("act", act(csq[:], pc[:], AF.Square, bias=pk1024[:, c, 1:2]), reads=[bpc, b_pk1024], writes=[bcsq])
                    S.op("pe", mmk(ps1[:], [(onesb[:], cuT[:, c, 512 * j:512 * (j + 1)])]), reads=[b_onesb, b_cuT[c][j]], writes=[bps1])
                    S.op("pe", mmk(ps2[:], [(onesb[:], csq[:])]), reads=[b_onesb, bcsq], writes=[bps2])
                    sl = slice(512 * j, 512 * (j + 1))
                    if c == 0:
                        S.op("dve", cp(ssum[:, sl], ps1[:]), reads=[bps1], writes=[b_ssum])
                        S.op("dve", cp(ssq[:, sl], ps2[:]), reads=[bps2], writes=[b_ssq])
                    else:
                        S.op("dve", tt(ssum[:, sl], ps1[:], ssum[:, sl], ALU.add), reads=[bps1, b_ssum], writes=[b_ssum])
                        S.op("dve", tt(ssq[:, sl], ps2[:], ssq[:, sl], ALU.add), reads=[bps2, b_ssq], writes=[b_ssq])
            S.op("act", act(ssum[:], ssum[:], AF.Copy, scale=1.0 / D), reads=[b_ssum], writes=[b_ssum])
            for j in range(4):
                t0, bt0 = tmp_bufs[j % 2]
                sl = slice(512 * j, 512 * (j + 1))
                S.op("dve", tt(t0[:], ssum[:, sl], ssum[:, sl], ALU.mult), reads=[b_ssum], writes=[bt0])
                S.op("dve", stt(t0[:], ssq[:, sl], 1.0 / D, t0[:], ALU.mult, ALU.subtract), reads=[b_ssq, bt0], writes=[bt0])
                S.op("act", act(ssq[:, sl], t0[:], AF.Sqrt, bias=EPS), reads=[bt0, b_ssq], writes=[b_ssq])
            S.op("dve", lambda E: E.reciprocal(out=ssq[:], in_=ssq[:]), reads=[b_ssq], writes=[b_ssq])
            cnt = 0
            for c in range(8):
                for j in range(4):
                    t1, bt1 = tmp_bufs[cnt % 2]
                    cnt += 1
                    sl = slice(512 * j, 512 * (j + 1))
                    S.op("dve", tt(t1[:], cuT[:, c, sl], ssum[:, sl], ALU.subtract), reads=[b_cuT[c][j], b_ssum], writes=[bt1])
                    S.op("dve", tt(t1[:], t1[:], ssq[:, sl], ALU.mult), reads=[bt1, b_ssq], writes=[bt1])
                    S.op("act", act(cuT[:, c, sl], t1[:], AF.Silu, scale=pk1024[:, c, 2:3], bias=pk1024[:, c, 3:4]),
                         reads=[bt1, b_pk1024], writes=[b_cuT[c][j]])
    with Scope(nc, S) as sc:
        wcv, b_wcv = sc.sb("wcv", [128, 8, D], BF16)
        wgc, b_wgc = sc.sb("wgc", [128, 8, D], BF16)
        S.dma("pool", dm(wcv[:], wview(w_cv_out, 0, D)), writes=[b_wcv])
        S.dma("pool", dm(wgc[:], wview(w_in, C_GCV, D)), writes=[b_wgc])
        m1_bufs = [sc.sb("m1s%d" % i, [128, 512], F32) for i in range(2)]
        sgt_bufs = [sc.sb("sgt%d" % i, [128, 512], F32) for i in range(2)]
        cnt = 0
        for c in range(8):
            for j in range(4):
                q = cnt % 2
                cnt += 1
                py, bpy = PB[2 * q]
                pg, bpg = PB[2 * q + 1]
                m1s, bm1s = m1_bufs[q]
                sgt, bsgt = sgt_bufs[q]
                sl = slice(512 * j, 512 * (j + 1))
                hs = slice(HALO + 512 * j, HALO + 512 * (j + 1))
                S.op("pe", mml([(py[:], wcv[:, k, c * 128:(c + 1) * 128], cuT[:, k, sl], k == 0, k == 7) for k in range(8)] +
                               [(pg[:], wgc[:, k, c * 128:(c + 1) * 128], n1T[:, k, hs], k == 0, k == 7) for k in range(8)]),
                     reads=[b_wcv, b_wgc] + n1_bufs_for(b_n1T, HALO + 512 * j, 512) + [b_cuT[k][j] for k in range(8)],
                     writes=[bpy, bpg])
                S.op("act", act(sgt[:], pg[:], AF.Sigmoid), reads=[bpg], writes=[bsgt])
                S.op("dve", tt(m1s[:], py[:], sgt[:], ALU.mult), reads=[bpy, bsgt], writes=[bm1s])
                S.dma("sp", dm(d_m1T[:, c, sl], m1s[:]), reads=[bm1s], writes=[b_dm1])
    LB.__exit__(None, None, None)
    if stage == "A":
        with Scope(nc, S) as sc:
            mt_, bmt_ = sc.sb("m1dbg", [128, 8, 512], F32)
            S.dma("sp", dm(mt_[:], d_m1T[:, :, 0:512]), reads=[b_dm1], writes=[bmt_])
            return dbg_finish([("o_m1T", mt_[:], [128, 8, 512], F32, [bmt_])])

    with Scope(nc, S) as sc:
        wso, b_wso = sc.sb("wso", [128, 16, D], BF16)
        wgs, b_wgs = sc.sb("wgs", [128, 8, D], BF16)
        S.dma("pool", dm(wso[:, 0:8, :], w_ssm_out[0:1024, :].rearrange("(k p) n -> p k n", p=128)), writes=[b_wso])
        S.dma("pool", dm(wso[:, 8:16, :], w_ssm_out[1024:2048, :].rearrange("(k p) n -> p k n", p=128)), writes=[b_wso])
        S.dma("pool", dm(wgs[:], wview(w_in, C_GSSM, D)), writes=[b_wgs])
        ynb_bufs = [sc.sb("ynb%d" % i, [128, 16, 512], BF16) for i in range(2)]
        sg_bufs = [sc.sb("sgc%d" % i, [128, 512], F32) for i in range(2)]
        mm_bufs = [sc.sb("mmc%d" % i, [128, 512], F32) for i in range(2)]
        ml_bufs = [sc.sb("mlc%d" % i, [128, 512], F32) for i in range(2)]
        mb_bufs = [sc.sb("mbc%d" % i, [128, 512], BF16) for i in range(2)]
        cnt = 0
        for j in range(4):
            ynb, bynb = ynb_bufs[j % 2]
            sl = slice(512 * j, 512 * (j + 1))
            hs = slice(HALO + 512 * j, HALO + 512 * (j + 1))
            S.dma("act", dm(ynb[:], d_ynT[:, :, sl]), reads=[b_dyn], writes=[bynb])
            for c in range(8):
                q = cnt % 2
                cnt += 1
                py, bpy = PB[2 * q]
                pg, bpg = PB[2 * q + 1]
                sg, bsg = sg_bufs[q]
                mm_, bmm = mm_bufs[q]
                ml, bml = ml_bufs[q]
                mb, bmb = mb_bufs[q]
                S.dma("sp", dm(ml[:], d_m1T[:, c, sl]), reads=[b_dm1], writes=[bml])
                S.op("pe", mml([(py[:], wso[:, k, c * 128:(c + 1) * 128], ynb[:, k, :], k == 0, k == 15) for k in range(16)] +
                               [(pg[:], wgs[:, k, c * 128:(c + 1) * 128], n1T[:, k, hs], k == 0, k == 7) for k in range(8)]),
                     reads=[b_wso, b_wgs, bynb] + n1_bufs_for(b_n1T, HALO + 512 * j, 512), writes=[bpy, bpg])
                S.op("act", act(sg[:], pg[:], AF.Sigmoid), reads=[bpg], writes=[bsg])
                S.op("dve", tt(mm_[:], py[:], sg[:], ALU.mult), reads=[bpy, bsg], writes=[bmm])
                S.op("pool", tt(mb[:], mm_[:], ml[:], ALU.add), reads=[bmm, bml], writes=[bmb])
                S.dma("sp", dm(d_mT[:, c, sl], mb[:]), reads=[bmb], writes=[b_dmT])
    if stage == "C1":
        with Scope(nc, S) as sc:
            mt_, bmt_ = sc.sb("mTdbg", [128, 8, 512], BF16)
            S.dma("sp", dm(mt_[:], d_mT[:, :, 0:512]), reads=[b_dmT], writes=[bmt_])
            return dbg_finish([("o_mT", mt_[:], [128, 8, 512], BF16, [bmt_])])
    LA.__exit__(None, None, None)

    LD = Scope(nc, S)
    LD.__enter__()
    n2T, _ = LD.sb("n2T", [128, 8, T], BF16)
    b_n2T = [Buf("n2T_%d" % i) for i in range(NT)]
    with Scope(nc, S) as sc:
        wmix, b_wmix = sc.sb("wmix", [128, 8, D], BF16)
        wr, b_wr = sc.sb("wr", [128, 8, 72], BF16)
        br, b_br = sc.sb("br", [128, 72], F32)
        S.dma("pool", dm(wmix[:], wview(w_mix_out, 0, D)), writes=[b_wmix])
        S.dma("pool", dm(wr[:], wr_d.rearrange("(k p) n -> p k n", p=128)), writes=[b_wr])
        S.dma("sp", dm(br[:], br_d), writes=[b_br])

        def tb2(name, shape, dtp, n=2):
            return [sc.sb("%s%d" % (name, i), shape, dtp) for i in range(n)]
        mt_b, xt_b, tm_b, ht_b, xn_b = tb2("mTt", [128, 8, 128], BF16), tb2("xtc", [128, D], F32), tb2("tmc", [128, D], F32), \
            tb2("htc", [128, D], F32), tb2("xnc", [128, D], F32)
        sq_t, b_sq = sc.sb("sqc", [128, D], BF16)
        st_b = tb2("stc", [128, 2], F32)
        n2f_b = tb2("n2f", [128, 8, 128], F32)
        xhi_b, xlo_b = tb2("xhi", [128, D], BF16), tb2("xlo", [128, D], BF16)
        lgall, b_lgall = sc.sb("lgall", [128, NT, 72], F32)
        gmx, _ = sc.sb("r_gmx", [128, NT], F32)
        gsum, _ = sc.sb("r_gsum", [128, NT], F32)
        m1_, _ = sc.sb("r_m1", [128, NT], F32)
        m2_, _ = sc.sb("r_m2", [128, NT], F32)
        dd, _ = sc.sb("r_dd", [128, NT], F32)
        w1_, _ = sc.sb("r_w1", [128, NT], F32)
        w2_, _ = sc.sb("r_w2", [128, NT], F32)
        ohg, _ = sc.sb("r_ohg", [128, NT, 8], F32)
        gsh, _ = sc.sb("r_gsh", [128, NT, 8], F32)
        pen, _ = sc.sb("r_pen", [128, NT, 8], F32)
        elm, _ = sc.sb("r_elm", [128, NT, 64], F32)
        elm2, _ = sc.sb("r_elm2", [128, NT, 64], F32)
        oh1, _ = sc.sb("r_oh1", [128, NT, 64], F32)
        oh2, _ = sc.sb("r_oh2", [128, NT, 64], F32)
        for i in range(NT):
            q = i % 2
            tsl = slice(i * 128, (i + 1) * 128)
            (mTt, bmTt), (xt, bxt), (tm, btm), (ht, bht), (xn, bxn) = mt_b[q], xt_b[q], tm_b[q], ht_b[q], xn_b[q]
            (st, bst), (n2f, bn2f) = st_b[q], n2f_b[q]
            S.dma("act", dm(mTt[:], d_mT[:, :, tsl]), reads=[b_dmT], writes=[bmTt])
            S.dma("sp", dm(xt[:], xh[HALO + i * 128:HALO + (i + 1) * 128, :]), writes=[bxt])
            for ob in range(2):
                pm, bpm = PB[ob]
                osl = slice(ob * 512, (ob + 1) * 512)
                S.op("pe", mmk(pm[:], [(mTt[:, k, :], wmix[:, k, osl]) for k in range(8)]), reads=[bmTt, b_wmix], writes=[bpm])
                S.op("dve", tt(tm[:, osl], pm[:], g1bc[:, osl], ALU.mult), reads=[bpm, b_g1bc], writes=[btm])
            S.op("dve", tt(ht[:], tm[:], xt[:], ALU.add), reads=[btm, bxt], writes=[bht])
            S.dma("sp", dm(d_h[tsl, :], ht[:]), reads=[bht], writes=[b_dh])
            if stage == "C2h":
                return dbg_finish([("o_ht", ht[:], [128, D], F32, [bht, b_dh])])
            S.op("act", act(sq_t[:], ht[:], AF.Square, scale=1.0 / 32, accum_out=st[:, 0:1]), reads=[bht], writes=[b_sq, bst])
            S.op("act", act(st[:, 1:2], st[:, 0:1], AF.Sqrt, bias=EPS), reads=[bst], writes=[bst])
            S.op("dve", lambda E, st=st: E.reciprocal(out=st[:, 1:2], in_=st[:, 1:2]), reads=[bst], writes=[bst])
            S.op("dve", ts(xn[:], ht[:], st[:, 1:2], ALU.mult), reads=[bht, bst], writes=[bxn])
            (xhi, bxhi), (xlo, bxlo) = xhi_b[q], xlo_b[q]
            S.op("dve", cp(xhi[:], xn[:]), reads=[bxn], writes=[bxhi])
            for k in range(8):
                tsa, ptb = TS[k]
                S.op("pe", mmk(tsa[:, 0:128], [(xhi[:, k * 128:(k + 1) * 128], identb[:])]), reads=[bxhi, b_identb], writes=[ptb])
                S.op("dve", ts(n2T[:, k, tsl], tsa[:, 0:128], s2col[:, k:k + 1], ALU.mult, modT[:, 24 + k:25 + k], ALU.add),
                     reads=[ptb, b_s2col, b_modT], writes=[b_n2T[i]])
            if stage == "C2a" and i == 0:
                return dbg_finish([("o_ht", ht[:], [128, D], F32, [bht]), ("o_n2", n2T[:, 0, 0:128], [128, 128], BF16, [b_n2T[0]])])
            pr, bpr = PB[2]
            S.op("pe", mmk(pr[:, 0:72], [(n2T[:, k, tsl], wr[:, k, :]) for k in range(8)]), reads=[b_n2T[i], b_wr], writes=[bpr])
            S.op("dve", tt(lgall[:, i, :], pr[:, 0:72], br[:], ALU.add), reads=[bpr, b_br], writes=[b_lgall])
        rw = dict(reads=[b_lgall], writes=[b_lgall])
        G = lgall[:, :, 0:8]
        EL = lgall[:, :, 8:72]
        S.op("dve", lambda E: E.tensor_reduce(out=gmx[:], in_=G, axis=AX.X, op=ALU.max), **rw)
        S.op("dve", tt(ohg[:], G, bc_in(gmx[:], 8), ALU.is_equal), **rw)
        S.op("dve", tt(gsh[:], G, bc_in(gmx[:], 8), ALU.subtract), **rw)
        S.op("act", act(gsh[:], gsh[:], AF.Exp), **rw)
        S.op("dve", lambda E: E.tensor_reduce(out=gsum[:], in_=gsh[:], axis=AX.X, op=ALU.add), **rw)
        S.op("dve", lambda E: E.reciprocal(out=gsum[:], in_=gsum[:]), **rw)
        S.op("dve", ts(pen[:], ohg[:], BIG, ALU.mult, -BIG, ALU.add), **rw)
        for gg in range(8):
            S.op("dve", tt(elm[:, :, gg * 8:(gg + 1) * 8], EL[:, :, gg * 8:(gg + 1) * 8], bc_in(pen[:, :, gg], 8), ALU.add), **rw)
        S.op("dve", lambda E: E.tensor_reduce(out=m1_[:], in_=elm[:], axis=AX.X, op=ALU.max), **rw)
        S.op("dve", tt(oh1[:], elm[:], bc_in(m1_[:], 64), ALU.is_equal), **rw)
        S.op("dve", stt(elm2[:], oh1[:], -BIG, elm[:], ALU.mult, ALU.add), **rw)
        S.op("dve", lambda E: E.tensor_reduce(out=m2_[:], in_=elm2[:], axis=AX.X, op=ALU.max), **rw)
        S.op("dve", tt(oh2[:], elm2[:], bc_in(m2_[:], 64), ALU.is_equal), **rw)
        S.op("dve", tt(dd[:], m2_[:], m1_[:], ALU.subtract), **rw)
        S.op("act", act(dd[:], dd[:], AF.Exp), **rw)
        S.op("dve", ts(w1_[:], dd[:], 1.0, ALU.add), **rw)
        S.op("dve", lambda E: E.reciprocal(out=w1_[:], in_=w1_[:]), **rw)
        S.op("dve", tt(w2_[:], dd[:], w1_[:], ALU.mult), **rw)
        S.op("dve", tt(oh1[:], oh1[:], bc_in(w1_[:], 64), ALU.mult), **rw)
        S.op("dve", tt(oh2[:], oh2[:], bc_in(w2_[:], 64), ALU.mult), **rw)
        S.op("dve", tt(oh1[:], oh1[:], oh2[:], ALU.add), **rw)
        S.op("dve", tt(gate[:], oh1[:], bc_in(gsum[:], 64), ALU.mult), reads=[b_lgall], writes=[b_gate])
    if stage == "C":
        with Scope(nc, S) as sc:
            hh, bhh = sc.sb("hall", [128, NT, D], F32)
            S.dma("sp", dm(hh[:], d_h.rearrange("(i p) d -> p i d", p=128)), reads=[b_dh], writes=[bhh])
            return dbg_finish([("o_h", hh[:], [128, NT, D], F32, [bhh]),
                               ("o_n2T", n2T[:], [128, 8, T], BF16, b_n2T),
                               ("o_gate", gate[:], [128, NT, 64], F32, [b_gate])])

    acc, _ = LD.sb("acc", [128, NT, D], F32)
    b_acc = [Buf("acc%d" % i) for i in range(NT)]
    with Scope(nc, S) as sc:
        w13_b = [sc.sb("w13_%d" % i, [128, 8, 1024], BF16) for i in range(2)]
        w2_b = [sc.sb("w2_%d" % i, [128, 4, 1024], BF16) for i in range(2)]
        hd_b = [sc.sb("hd%d" % i, [128, 4, 512], BF16) for i in range(2)]
        s1_b = [sc.sb("s1m%d" % i, [128, 512], F32) for i in range(2)]
        cnt = 0
        hcnt = 0
        ocnt = 0
        for e in range(nexp):
            w13, bw13 = w13_b[e % 2]
            w2b, bw2 = w2_b[e % 2]
            S.dma("pool", dm(w13[:, :, 0:512], w1_d[e].rearrange("(k p) n -> p k n", p=128)), writes=[bw13])
            S.dma(MOEQ[1], dm(w13[:, :, 512:1024], w3_d[e].rearrange("(k p) n -> p k n", p=128)), writes=[bw13])
            S.dma(MOEQ[2], dm(w2b[:], w2_d[e].rearrange("(k p) n -> p k n", p=128)), writes=[bw2])
            for j in range(4):
                hd, bhd = hd_b[hcnt % 2]
                hcnt += 1
                sl = slice(512 * j, 512 * (j + 1))
                rn = [b_n2T[4 * j + t_] for t_ in range(4)]
                for fc in range(4):
                    q = cnt % 2
                    cnt += 1
                    p1, bp1 = PB[2 * q]
                    p3, bp3 = PB[2 * q + 1]
                    s1m, bs1 = s1_b[q]
                    S.op("pe", mml([(p1[:], w13[:, k, fc * 128:(fc + 1) * 128], n2T[:, k, sl], k == 0, k == 7) for k in range(8)] +
                                   [(p3[:], w13[:, k, 512 + fc * 128:512 + (fc + 1) * 128], n2T[:, k, sl], k == 0, k == 7)
                                    for k in range(8)]), reads=[bw13] + rn, writes=[bp1, bp3])
                    S.op("act", act(s1m[:], p1[:], AF.Silu), reads=[bp1], writes=[bs1])
                    S.op("dve", tt(hd[:, fc, :], s1m[:], p3[:], ALU.mult), reads=[bs1, bp3], writes=[bhd])
                for t_ in range(4):
                    i = 4 * j + t_
                    for ob in range(2):
                        po, bpo = PB[4 + ocnt % 4]
                        ocnt += 1
                        osl = slice(ob * 512, (ob + 1) * 512)
                        S.op("pe", mmk(po[:], [(hd[:, fc, t_ * 128:(t_ + 1) * 128], w2b[:, fc, osl]) for fc in range(4)]),
                             reads=[bhd, bw2], writes=[bpo])
                        if e == 0:
                            S.op("dve", ts(acc[:, i, osl], po[:], gate[:, i, e:e + 1], ALU.mult),
                                 reads=[bpo, b_gate], writes=[b_acc[i]])
                        else:
                            S.op("dve", stt(acc[:, i, osl], po[:], gate[:, i, e:e + 1], acc[:, i, osl], ALU.mult, ALU.add),
                                 reads=[bpo, b_gate, b_acc[i]], writes=[b_acc[i]])

    with Scope(nc, S) as sc:
        fg, b_fg = sc.sb("fg", [128, D], F32)
        S.dma("sp", dm(fg[:], fg_d), writes=[b_fg])
        ht_b = [sc.sb("hte%d" % i, [128, D], F32) for i in range(2)]
        o_b = [sc.sb("oe%d" % i, [128, D], F32) for i in range(2)]
        o2_b = [sc.sb("o2e%d" % i, [128, D], F32) for i in range(2)]
        sq_t, b_sq = sc.sb("sqe", [128, D], BF16)
        st_b = [sc.sb("ste%d" % i, [128, 2], F32) for i in range(2)]
        for i in range(NT):
            q = i % 2
            (ht, bht), (o, bo), (o2, bo2), (st, bst) = ht_b[q], o_b[q], o2_b[q], st_b[q]
            tsl = slice(i * 128, (i + 1) * 128)
            S.dma("sp", dm(ht[:], d_h[tsl, :]), reads=[b_dh], writes=[bht])
            S.op("dve", tt(o[:], acc[:, i, :], g2bc[:], ALU.mult), reads=[b_acc[i], b_g2bc], writes=[bo])
            S.op("dve", tt(o[:], o[:], ht[:], ALU.add), reads=[bo, bht], writes=[bo])
            S.op("act", act(sq_t[:], o[:], AF.Square, scale=1.0 / 32, accum_out=st[:, 0:1]), reads=[bo], writes=[b_sq, bst])
            S.op("act", act(st[:, 1:2], st[:, 0:1], AF.Sqrt, bias=EPS), reads=[bst], writes=[bst])
            S.op("dve", lambda E, st=st: E.reciprocal(out=st[:, 1:2], in_=st[:, 1:2]), reads=[bst], writes=[bst])
            S.op("dve", stt(o2[:], o[:], st[:, 1:2], fg[:], ALU.mult, ALU.mult), reads=[bo, bst, b_fg], writes=[bo2])
            S.dma("sp", dm(out_d[tsl, :], o2[:]), reads=[bo2], writes=[b_out])
    S.finish([b_out])
    return nc


def prep_inputs(inp):
    f = {k: np.asarray(v, dtype=np.float32) for k, v in inp.items()}
    x = f["x"][0]

    def fm(v, nch):
        v = np.atleast_2d(v)
        return np.ascontiguousarray(v.reshape(v.shape[0], nch, 128).transpose(2, 1, 0))

    def rep(v):
        return np.ascontiguousarray(np.broadcast_to(np.asarray(v, np.float32)[None, :], (128, v.shape[-1])))
    rows1024 = np.concatenate([f["norm1_g"][0][None], f["cv_dw_b"][0][None], f["cv_ln_g"][0][None],
                               f["cv_ln_b"][0][None], f["norm2_g"][0][None], f["final_g"][None],
                               f["cv_dw_w"][0]], axis=0)
    rows3072 = np.concatenate([f["ssm_conv_b"][0][None], f["ssm_conv_w"][0]], axis=0)
    t_i = np.arange(128)[:, None]
    s_i = np.arange(128)[None, :]
    tri = np.stack([(t_i > s_i), (t_i <= s_i), np.where(s_i < t_i, NEGV, 0.0)], axis=1).astype(np.float32)
    xpad = np.zeros(((NCORES - 1) * T + HALO, D), np.float32)
    xpad[HALO:] = x[:(NCORES - 1) * T]
    common = {
        "xpad": xpad,
        "cT": np.ascontiguousarray(f["c"][0].reshape(8, 128).T),
        "w_ada": f["w_ada"][0], "b_ada": f["b_ada"][0][None], "w_in": f["w_in"][0],
        "pk1024": fm(rows1024, 8), "pk3072": fm(rows3072, 24),
        "w_cv_out": f["w_cv_out"][0],
        "ident": np.eye(128, dtype=np.float32),
        "tri": np.ascontiguousarray(tri),
        "rows32": np.concatenate([rep(f["dt_bias"][0]), rep(f["a_log"][0]), rep(f["d_skip"][0])], axis=1),
        "gn": np.ascontiguousarray(fm(f["ssm_norm_g"][0][None], 16)[:, :, 0]),
        "w_ssm_out": f["w_ssm_out"][0], "w_mix_out": f["w_mix_out"][0],
        "wr": np.ascontiguousarray(np.concatenate([f["w_grp"][0], f["w_er"][0]], axis=1)),
        "br": rep(np.concatenate([f["b_grp"][0], f["b_er"][0]])),
        "w1": f["w1"][0], "w3": f["w3"][0], "w2": f["w2"][0],
        "fg": rep(f["final_g"]),
    }
    maps = []
    for k in range(NCORES):
        xk = np.zeros((TH, D), np.float32)
        xk[HALO:] = x[k * T:(k + 1) * T]
        if k > 0:
            xk[:HALO] = x[k * T - HALO:k * T]
        m = dict(common)
        m["xh"] = xk
        m["hmask"] = np.full((128, 1), 0.0 if k == 0 else 1.0, np.float32)
        m["segmask"] = np.ascontiguousarray(np.broadcast_to((np.arange(8) < k).astype(np.float32)[None, :], (128, 8)))
        maps.append(m)
    return maps


def kernel(**inputs):
    maps = prep_inputs(inputs)
    nc = build_program("full")
    res = run_bass_kernel_spmd(nc, maps, core_ids=list(range(NCORES)))
    out = np.concatenate([np.asarray(res.results[k]["out"], np.float32) for k in range(NCORES)], axis=0)
    return out.reshape(1, SEQ, D)
```
